# Optimizing a Trainium2 kernel written in Bass

```python
import jax, jax.numpy as jnp
from jax import lax
import numpy as np

D_MODEL = 2048
BATCH = 16
SEQ = 2048
DEPTH = 4

D_FF = 4 * D_MODEL
CONV_WIDTH = 3
N_HEADS = 16
QK_HEAD_DIM = 128
V_HEAD_DIM = 128
Q_LORA = 512
KV_LORA = 256
IDX_HEADS = 16
IDX_DIM = 64
TOPK_MAX = 256
Q_BLOCK = 64
ATTN_SCALE = QK_HEAD_DIM ** -0.5
IN_COLS = Q_LORA + KV_LORA + IDX_DIM + IDX_HEADS
EPS = 1e-6
N_CONV = (DEPTH + 1) // 2
N_ATTN = DEPTH // 2

kernel_name = "hybrid_shortconv_dsa_sqrelu"


def rmsnorm(x, g):
    xf = x.astype(jnp.float32)
    y = xf * lax.rsqrt(jnp.mean(xf * xf, axis=-1, keepdims=True) + EPS)
    return (y * g.astype(jnp.float32)).astype(x.dtype)


def layernorm(x, g, b):
    xf = x.astype(jnp.float32)
    mu = jnp.mean(xf, axis=-1, keepdims=True)
    var = jnp.mean(jnp.square(xf - mu), axis=-1, keepdims=True)
    y = (xf - mu) * lax.rsqrt(var + EPS)
    return y * g.astype(jnp.float32) + b.astype(jnp.float32)


def sq_relu_mlp(h, w1, w2):
    return jnp.square(jax.nn.relu(h @ w1)) @ w2


def short_conv_mixer(h, w_in, conv_w, w_out):
    bg, cg, xt = jnp.split(h @ w_in, 3, axis=-1)
    z = cg * xt
    zp = jnp.pad(z, ((0, 0), (CONV_WIDTH - 1, 0), (0, 0)))
    s = z.shape[1]
    zc = conv_w[0] * zp[:, 0:s] + conv_w[1] * zp[:, 1:s + 1] + conv_w[2] * zp[:, 2:s + 2]
    return (bg * zc) @ w_out


def dsa_mixer(h, w_in, q_g, kv_g, w_uq, w_uk, w_uv, w_qidx, ln_g, ln_b, w_out):
    b, s, _ = h.shape
    topk = min(TOPK_MAX, s // 4)
    proj = h @ w_in
    c_q, c_kv, k_idx, w_idx = jnp.split(
        proj, [Q_LORA, Q_LORA + KV_LORA, Q_LORA + KV_LORA + IDX_DIM], axis=-1)
    c_q = rmsnorm(c_q, q_g)
    c_kv = rmsnorm(c_kv, kv_g)
    k_idx = layernorm(k_idx, ln_g, ln_b)
    w_idx = w_idx.astype(jnp.float32) * (IDX_HEADS ** -0.5 * IDX_DIM ** -0.5)
    nb = s // Q_BLOCK
    key_pos = jnp.arange(s, dtype=jnp.int32)

    def to_blocks(a):
        return a.reshape(b, nb, Q_BLOCK, *a.shape[2:]).swapaxes(0, 1)

    def block(args):
        cq, wi, t = args
        q = jnp.einsum('bqr,rhd->bqhd', cq, w_uq)
        q_lat = jnp.einsum('bqhd,hdc->bqhc', q, w_uk)
        q_idx = jnp.einsum('bqr,rhe->bqhe', cq, w_qidx).astype(jnp.float32)
        rel = jax.nn.relu(jnp.einsum('bqhe,bse->bqhs', q_idx, k_idx))
        score = jnp.einsum('bqhs,bqh->bqs', rel, wi)
        causal = key_pos[None, :] <= t[:, None]
        score = jnp.where(causal[None], score, -jnp.inf)
        _, idx = lax.top_k(score, topk)
        kv_sel = jax.vmap(lambda c, i: c[i])(c_kv, idx)
        logits = jnp.einsum('bqhc,bqkc->bqhk', q_lat, kv_sel).astype(jnp.float32) * ATTN_SCALE
        valid = (idx <= t[None, :, None])[:, :, None, :]
        p = jax.nn.softmax(jnp.where(valid, logits, -jnp.inf), axis=-1).astype(h.dtype)
        o_lat = jnp.einsum('bqhk,bqkc->bqhc', p, kv_sel)
        o = jnp.einsum('bqhc,hcv->bqhv', o_lat, w_uv).reshape(b, Q_BLOCK, N_HEADS * V_HEAD_DIM)
        return o @ w_out

    out = lax.map(block, (to_blocks(c_q), to_blocks(w_idx), key_pos.reshape(nb, Q_BLOCK)))
    return out.swapaxes(0, 1).reshape(b, s, -1)


def setup_inputs(seed: int = 0) -> dict:
    key = jax.random.key(seed)
    ks = jax.random.split(key, 20)
    f32 = jnp.float32

    def nrm(k, shape, fan_in):
        return jax.random.normal(k, shape, f32) * (fan_in ** -0.5)

    def gain(k, shape):
        return 1.0 + 0.02 * jax.random.normal(k, shape, f32)

    return {
        "x": jax.random.normal(ks[0], (BATCH, SEQ, D_MODEL), f32),
        "norm_mix": gain(ks[1], (DEPTH, D_MODEL)),
        "norm_mlp": gain(ks[2], (DEPTH, D_MODEL)),
        "mlp_w1": nrm(ks[3], (DEPTH, D_MODEL, D_FF), D_MODEL),
        "mlp_w2": nrm(ks[4], (DEPTH, D_FF, D_MODEL), D_FF),
        "conv_in": nrm(ks[5], (N_CONV, D_MODEL, 3 * D_MODEL), D_MODEL),
        "conv_w": nrm(ks[6], (N_CONV, CONV_WIDTH, D_MODEL), CONV_WIDTH),
        "conv_out": nrm(ks[7], (N_CONV, D_MODEL, D_MODEL), D_MODEL),
        "attn_in": nrm(ks[8], (N_ATTN, D_MODEL, IN_COLS), D_MODEL),
        "q_norm": gain(ks[9], (N_ATTN, Q_LORA)),
        "kv_norm": gain(ks[10], (N_ATTN, KV_LORA)),
        "w_uq": nrm(ks[11], (N_ATTN, Q_LORA, N_HEADS, QK_HEAD_DIM), Q_LORA),
        "w_uk": nrm(ks[12], (N_ATTN, N_HEADS, QK_HEAD_DIM, KV_LORA), QK_HEAD_DIM),
        "w_uv": nrm(ks[13], (N_ATTN, N_HEADS, KV_LORA, V_HEAD_DIM), KV_LORA),
        "w_qidx": nrm(ks[14], (N_ATTN, Q_LORA, IDX_HEADS, IDX_DIM), Q_LORA),
        "kidx_ln_g": gain(ks[15], (N_ATTN, IDX_DIM)),
        "kidx_ln_b": 0.02 * jax.random.normal(ks[16], (N_ATTN, IDX_DIM), f32),
        "attn_out": nrm(ks[17], (N_ATTN, N_HEADS * V_HEAD_DIM, D_MODEL), N_HEADS * V_HEAD_DIM),
        "final_norm": gain(ks[18], (D_MODEL,)),
    }


def reference(x, norm_mix, norm_mlp, mlp_w1, mlp_w2, conv_in, conv_w, conv_out,
              attn_in, q_norm, kv_norm, w_uq, w_uk, w_uv, w_qidx, kidx_ln_g, kidx_ln_b,
              attn_out, final_norm):
    h = x
    for i in range(DEPTH):
        j = i // 2
        hn = rmsnorm(h, norm_mix[i])
        if i % 2 == 0:
            h = h + short_conv_mixer(hn, conv_in[j], conv_w[j], conv_out[j])
        else:
            h = h + dsa_mixer(hn, attn_in[j], q_norm[j], kv_norm[j], w_uq[j], w_uk[j],
                              w_uv[j], w_qidx[j], kidx_ln_g[j], kidx_ln_b[j], attn_out[j])
        h = h + sq_relu_mlp(rmsnorm(h, norm_mlp[i]), mlp_w1[i], mlp_w2[i])
    return rmsnorm(h, final_norm)
```

```python
from contextlib import ExitStack
import numpy as np
import concourse.bass as bass
import concourse.mybir as mybir
from concourse.bass_utils import run_bass_kernel_spmd

F32 = mybir.dt.float32
BF16 = mybir.dt.bfloat16
U8 = mybir.dt.uint8
ALU = mybir.AluOpType
AF = mybir.ActivationFunctionType
AX = mybir.AxisListType

D = 2048
DFF = 8192
SEQ = 2048
NCORES = 8
TT = 512
EPS = 1e-6
NEG = -1.0e30
SLOT_BYTES = 16384
NSLOT = 3
CELL = 256
SAME_ENG_SYNC = True
ESZ = {F32: 4, BF16: 2, U8: 1}


class T:
    __slots__ = ("ap", "space", "lo", "hi", "esz")

    def __init__(self, ap, space, lo, hi, esz):
        self.ap, self.space, self.lo, self.hi, self.esz = ap, space, lo, hi, esz

    def idx(self, i):
        n = self.ap.shape[1]
        sz = (self.hi - self.lo) // n
        return T(self.ap[:, i], self.space, self.lo + i * sz, self.lo + (i + 1) * sz, self.esz)

    def cols(self, a, b):
        assert len(self.ap.shape) == 2
        return T(self.ap[:, a:b], self.space, self.lo + a * self.esz, self.lo + b * self.esz, self.esz)

    def parts(self, a, b):
        return T(self.ap[a:b], self.space, self.lo, self.hi, self.esz)

    def view(self, ap):
        return T(ap, self.space, self.lo, self.hi, self.esz)


class DR:
    __slots__ = ("ap", "key")

    def __init__(self, ap, key):
        self.ap, self.key = ap, key


class Op:
    __slots__ = ("eng", "fn", "deps", "key", "seq", "sig", "val", "dsem")


class Prog:
    ENGS = ("pe", "act", "dve", "pool", "sp")

    def __init__(self, nc):
        self.nc = nc
        self.lists = {e: [] for e in self.ENGS}
        self.cells = {}
        self.nseq = {e: 0 for e in self.ENGS}
        self.dma_count = {}
        self.out_dma = {}

    def _cells(self, t):
        if isinstance(t, DR):
            return [("dram", t.key)]
        if t.space == "ps":
            return [("ps", c) for c in range(t.lo // 2048, (t.hi + 2047) // 2048)]
        return [("sb", c) for c in range(t.lo // CELL, (t.hi + CELL - 1) // CELL)]

    def add(self, eng, fn, reads=(), writes=(), dsem=None, is_out=False):
        op = Op()
        op.eng, op.fn, op.sig, op.val, op.dsem = eng, fn, False, 0, dsem
        if dsem is not None:
            op.key = ("dma", dsem)
            self.dma_count[dsem] = self.dma_count.get(dsem, 0) + 16
            op.seq = self.dma_count[dsem]
            if is_out:
                self.out_dma[dsem] = op.seq
        else:
            op.key = eng
            self.nseq[eng] += 1
            op.seq = self.nseq[eng]
        deps = {}

        def need(o):
            if o is None or o is op:
                return
            if o.dsem is None and op.dsem is None and o.eng == eng:
                if eng == "pe" or not SAME_ENG_SYNC:
                    return
            cur = deps.get(o.key)
            if cur is None or cur.seq < o.seq:
                deps[o.key] = o

        for t in reads:
            for c in self._cells(t):
                st = self.cells.get(c)
                if st is None:
                    st = self.cells[c] = [None, {}]
                need(st[0])
                cur = st[1].get(op.key)
                if cur is None or cur.seq < op.seq:
                    st[1][op.key] = op
        for t in writes:
            for c in self._cells(t):
                st = self.cells.get(c)
                if st is None:
                    st = self.cells[c] = [None, {}]
                need(st[0])
                for r in st[1].values():
                    need(r)
                st[0] = op
                st[1] = {}
        op.deps = list(deps.values())
        for o in op.deps:
            if o.dsem is None:
                o.sig = True
        self.lists[eng].append(op)
        return op

    def emit(self, stack):
        nc = self.nc
        sems = {e: stack.enter_context(nc.semaphore("sem_" + e)) for e in self.ENGS}
        dsems = {k: stack.enter_context(nc.semaphore("dsem_%s" % str(k))) for k in self.dma_count}
        for e in self.ENGS:
            cnt = 0
            for op in self.lists[e]:
                if op.dsem is None and op.sig:
                    cnt += 1
                    op.val = cnt
        block = stack.enter_context(nc.Block())

        def run(ename, eh):
            known = {}
            for op in self.lists[ename]:
                for o in op.deps:
                    if o.dsem is None:
                        s, v = sems[o.eng], o.val
                    else:
                        s, v = dsems[o.dsem], o.seq
                    if known.get(id(s), 0) >= v:
                        continue
                    known[id(s)] = v
                    eh.wait_ge(s, v)
                ins = op.fn(eh)
                if op.dsem is not None:
                    ins.then_inc(dsems[op.dsem], 16)
                elif op.sig:
                    ins.then_inc(sems[ename], 1)
            if ename == "sp":
                for k, v in self.out_dma.items():
                    eh.wait_ge(dsems[k], v)

        @block.tensor
        def _(e):
            run("pe", e)

        @block.scalar
        def _(e):
            run("act", e)

        @block.vector
        def _(e):
            run("dve", e)

        @block.gpsimd
        def _(e):
            run("pool", e)

        @block.sync
        def _(e):
            run("sp", e)


def vec_layout():
    cols = {}
    n = 0

    def put(name, k):
        nonlocal n
        cols[name] = n
        n += k
    for i in range(4):
        put(("norm_mix", i), 16)
        put(("norm_mlp", i), 16)
    put(("final_norm",), 16)
    for j in range(2):
        for k in range(3):
            put(("conv_w", j, k), 16)
        put(("q_norm", j), 4)
        put(("kv_norm", j), 2)
        put(("ln_g", j), 1)
        put(("ln_b", j), 1)
    return cols, n


def const_layout():
    cols = {}
    n = 0

    def put(name, k):
        nonlocal n
        cols[name] = n
        n += k
    put("ident", 128)
    put("ones", 128)
    for s in range(4):
        put(("cb", s), 512)
    put("m8", 8)
    put("d8", 8)
    put("bsel", 16)
    return cols, n


def build_vecs(inp):
    cols, n = vec_layout()
    v = np.zeros((128, n), np.float32)

    def setv(name, arr):
        arr = np.asarray(arr, np.float32).reshape(-1)
        k = arr.size // 128
        if k == 0:
            v[:arr.size, cols[name]] = arr
        else:
            v[:, cols[name]:cols[name] + k] = arr.reshape(k, 128).T
    for i in range(4):
        setv(("norm_mix", i), inp["norm_mix"][i])
        setv(("norm_mlp", i), inp["norm_mlp"][i])
    setv(("final_norm",), inp["final_norm"])
    for j in range(2):
        for k in range(3):
            setv(("conv_w", j, k), inp["conv_w"][j, k])
        setv(("q_norm", j), inp["q_norm"][j])
        setv(("kv_norm", j), inp["kv_norm"][j])
        setv(("ln_g", j), inp["kidx_ln_g"][j])
        setv(("ln_b", j), inp["kidx_ln_b"][j])
    return v


def build_consts():
    cols, n = const_layout()
    c = np.zeros((128, n), np.float32)
    p = np.arange(128)
    c[:, cols["ident"]:cols["ident"] + 128] = np.eye(128, dtype=np.float32)
    c[:, cols["ones"]:cols["ones"] + 128] = 1.0
    s = np.arange(512)
    for st in range(4):
        vis = s[None, :] <= (st * 128 + p)[:, None]
        c[:, cols[("cb", st)]:cols[("cb", st)] + 512] = np.where(vis, 0.0, NEG)
    c[:, cols["m8"]:cols["m8"] + 8] = (p[:, None] % 8 == np.arange(8)[None, :])
    c[:, cols["d8"]:cols["d8"] + 8] = (p[:, None] // 16 == np.arange(8)[None, :])
    c[:, cols["bsel"]:cols["bsel"] + 16] = (p[:, None] // 8 == np.arange(16)[None, :])
    return c


class Builder:
    def __init__(self, nseq=2, layers=(0, 1, 2, 3), do_final=True, debug=None, ntile=None):
        self.nseq = nseq
        self.ntok = nseq * SEQ
        self.ntile = ntile or self.ntok // TT
        self.layers = tuple(layers)
        self.do_final = do_final
        self.debug = debug
        self.nc = bass.Bass("TRN2", target_bir_lowering=False)
        self.P = Prog(self.nc)
        self.vcols, self.nv = vec_layout()
        self.ccols, self.ncc = const_layout()
        self.sb_off = 0
        self.ps_rr = 0
        self.pieces = []
        self.use_order = []
        self.dsem_n = 0

    def new_dsem(self):
        self.dsem_n += 1
        return self.dsem_n

    def sb(self, nbytes, dtype, shape=None, at=None):
        esz = ESZ[dtype]
        if at is None:
            lo = (self.sb_off + 63) // 64 * 64
            self.sb_off = lo + nbytes
            assert self.sb_off <= self.arena_bytes, ("SBUF overflow", self.sb_off)
        else:
            lo = at
        ap = self.arena[:, lo:lo + nbytes].bitcast(dtype)
        if shape is not None and len(shape) == 2:
            ap = ap.rearrange("p (a b) -> p a b", b=shape[1])
        elif shape is not None and len(shape) == 3:
            ap = ap.rearrange("p (a b c) -> p a b c", b=shape[1], c=shape[2])
        return T(ap, "sb", lo, lo + nbytes, esz)

    def bank(self, i, dtype=F32):
        ap = self.psum[:, i * 512:(i + 1) * 512]
        if dtype != F32:
            ap = ap.bitcast(dtype)
        return T(ap, "ps", i * 2048, (i + 1) * 2048, ESZ[dtype])

    def next_bank(self, dtype=F32):
        b = self.bank(self.ps_rr % 8, dtype)
        self.ps_rr += 1
        return b

    def mm(self, out, lhsT, rhs, start, stop):
        self.P.add("pe", lambda e: e.matmul(out.ap, lhsT.ap, rhs.ap, start=start, stop=stop),
                   reads=[lhsT, rhs], writes=[out])

    def tr(self, out, in_, ident):
        self.P.add("pe", lambda e: e.transpose(out.ap, in_.ap, ident.ap), reads=[in_, ident], writes=[out])

    def act(self, out, in_, func, scale=1.0, bias=0.0, extra_reads=()):
        self.P.add("act", lambda e: e.activation(out.ap, in_.ap, func, bias=bias, scale=scale),
                   reads=[in_] + list(extra_reads), writes=[out])

    def tt(self, eng, out, a, b, op):
        h = "dve" if eng == "dve" else "pool"
        self.P.add(h, lambda e: e.tensor_tensor(out.ap, a.ap, b.ap, op), reads=[a, b], writes=[out])

    def ts(self, eng, out, a, s1, s2, op0, op1=None, reads=(), accum=None):
        kw = {}
        if op1 is not None:
            kw["op1"] = op1
        w = [out]
        if accum is not None:
            kw["accum_out"] = accum.ap
            w.append(accum)
        self.P.add(eng, lambda e: e.tensor_scalar(out.ap, a.ap, s1, s2, op0, **kw),
                   reads=[a] + list(reads), writes=w)

    def stt(self, out, a, scalar, b, op0, op1, reads=()):
        self.P.add("dve", lambda e: e.scalar_tensor_tensor(out.ap, a.ap, scalar, b.ap, op0, op1),
                   reads=[a, b] + list(reads), writes=[out])

    def copy(self, eng, out, in_):
        if eng == "act":
            self.P.add("act", lambda e: e.copy(out.ap, in_.ap), reads=[in_], writes=[out])
        else:
            self.P.add(eng, lambda e: e.tensor_copy(out.ap, in_.ap), reads=[in_], writes=[out])

    def dma(self, q, out, in_, dsem, reads, writes, is_out=False):
        self.P.add(q, lambda e: e.dma_start(out=out, in_=in_), reads=reads, writes=writes, dsem=dsem,
                   is_out=is_out)

    def vcol(self, name, c=0, n=1, parts=128):
        o = self.vcols[name] + c
        return self.vecs.ap[0:parts, o:o + n]

    def add_piece(self, src_ap, shape):
        n = int(np.prod(shape[1:]))
        assert n * 2 <= SLOT_BYTES
        idx = len(self.pieces)
        scr = self.nc.dram_tensor("wscr%d" % idx, [128, n], BF16, kind="Internal").ap()
        res = DR(scr, ("w", idx))
        sem = 100 + (idx % 2)
        if len(shape) == 3:
            self.dma("pool", scr.rearrange("p (a b) -> p a b", b=shape[2]), src_ap, sem, reads=[], writes=[res])
        else:
            sub = shape[2] * shape[3]
            for g in range(shape[1]):
                dst = scr[:, g * sub:(g + 1) * sub].rearrange("p (a b) -> p a b", b=shape[3])
                self.dma("pool", dst, src_ap[g], sem, reads=[], writes=[DR(None, ("w", idx, g))])
        self.pieces.append((res, n, shape))
        return idx

    def wget(self, idx, hold=False):
        pos = self.wpos
        assert self.use_order[pos] == idx, (pos, idx, self.use_order[pos])
        if hold and self.whold is None:
            self.whold = pos
        base = pos if self.whold is None else self.whold
        while self.wissued < min(len(self.use_order), base + NSLOT):
            k = self.wissued
            res, n, shape = self.pieces[self.use_order[k]]
            s = k % NSLOT
            st = self.slots[s]
            dst = st.ap[:, 0:n]
            rr = [res] if len(shape) == 3 else [DR(None, ("w", self.use_order[k], g)) for g in range(shape[1])]
            self.dma("sp", dst, res.ap, 200 + s, reads=rr, writes=[T(dst, "sb", st.lo, st.lo + 2 * n, 2)])
            self.wissued += 1
        self.wpos += 1
        res, n, shape = self.pieces[idx]
        st = self.slots[pos % NSLOT]
        ap = st.ap[:, 0:n]
        if len(shape) == 3:
            ap = ap.rearrange("p (a b) -> p a b", b=shape[2])
        elif len(shape) == 4:
            ap = ap.rearrange("p (a b c) -> p a b c", b=shape[2], c=shape[3])
        return T(ap, "sb", st.lo, st.lo + 2 * n, 2)

    def rmsnorm(self, src, nch, gname, inv_n, dst, ones_parts=128, width=TT):
        ps = self.next_bank()
        for c in range(nch):
            sq = self.sqt[c % 2]
            self.act(sq, src.idx(c), AF.Square)
            self.mm(ps, self.ones_bf, sq, c == 0, c == nch - 1)
        self.act(self.rstd, ps, AF.Ln, scale=inv_n, bias=self.eps_t.ap, extra_reads=[self.eps_t])
        self.act(self.rstd, self.rstd, AF.Exp, scale=-0.5)
        for c in range(nch):
            self.stt(dst.idx(c), src.idx(c), self.vcol(gname, c), self.rstd, ALU.mult, ALU.mult,
                     reads=[self.vecs])

    def mlp(self, layer):
        R, xn, h = self.R, self.xn, self.hbuf
        self.rmsnorm(R, 16, ("norm_mlp", layer), 1.0 / D, xn)
        for hh in range(2):
            for fg in range(hh * 8, hh * 8 + 8):
                slot = self.wget(self.pc[("w1", layer, fg)])
                for jj in range(4):
                    j = (fg - hh * 8) * 4 + jj
                    ps = self.next_bank()
                    for kc in range(16):
                        self.mm(ps, slot.idx(kc).cols(jj * 128, (jj + 1) * 128), xn.idx(kc), kc == 0, kc == 15)
                    tmp = self.rtmp[j % 2]
                    self.act(tmp, ps, AF.Relu)
                    self.tt("dve", h.idx(j), tmp, tmp, ALU.mult)
            for half in range(2):
                banks = [self.bank(i) for i in range(8)]
                for kg in range(hh * 4, hh * 4 + 4):
                    slot = self.wget(self.pc[("w2", layer, half, kg)])
                    for kk in range(8):
                        k = (kg - hh * 4) * 8 + kk
                        for dl in range(8):
                            self.mm(banks[dl], slot.idx(kk).cols(dl * 128, (dl + 1) * 128), h.idx(k), k == 0, k == 31)
                for dl in range(8):
                    d = half * 8 + dl
                    self.tt("dve", R.idx(d), banks[dl], R.idx(d), ALU.add)
        self.ps_rr = 0

    def conv_mixer(self, layer, first_in_seq):
        j = layer // 2
        R, xn = self.R, self.xn
        u = self.sb(16 * TT * 2, BF16, [16, TT], at=self.hbuf.lo)
        self.rmsnorm(R, 16, ("norm_mix", layer), 1.0 / D, xn)
        for dj in range(16):
            slot = self.wget(self.pc[("cin", j, dj)])
            bks = [self.next_bank() for _ in range(3)]
            for g in range(3):
                for kc in range(16):
                    lw = slot.view(slot.ap[:, g, kc, :])
                    self.mm(bks[g], lw, xn.idx(kc), kc == 0, kc == 15)
            z = self.zt[dj % 2]
            cgs = self.cgs[dj % 2]
            acc = self.acc[dj % 2]
            halo = self.halo.idx(dj)
            self.copy("act", cgs, bks[1])
            if first_in_seq:
                self.P.add("pool", lambda e, z=z: e.memset(z.ap[:, 0:2], 0.0), writes=[z.cols(0, 2)])
            else:
                self.copy("pool", z.cols(0, 2), halo)
            self.tt("dve", z.cols(2, TT + 2), bks[2], cgs, ALU.mult)
            self.ts("dve", acc, z.cols(2, TT + 2), self.vcol(("conv_w", j, 2), dj), None, ALU.mult,
                    reads=[self.vecs])
            self.stt(acc, z.cols(1, TT + 1), self.vcol(("conv_w", j, 1), dj), acc, ALU.mult, ALU.add,
                     reads=[self.vecs])
            self.stt(acc, z.cols(0, TT), self.vcol(("conv_w", j, 0), dj), acc, ALU.mult, ALU.add,
                     reads=[self.vecs])
            self.copy("pool", halo, z.cols(TT, TT + 2))
            self.tt("dve", u.idx(dj), bks[0], acc, ALU.mult)
        for og in range(4):
            slot = self.wget(self.pc[("cout", j, og)])
            for dd in range(4):
                d = og * 4 + dd
                ps = self.next_bank()
                for kc in range(16):
                    self.mm(ps, slot.idx(kc).cols(dd * 128, (dd + 1) * 128), u.idx(kc), kc == 0, kc == 15)
                self.tt("dve", R.idx(d), ps, R.idx(d), ALU.add)

    def load_x_tile(self, tile):
        xin = self.sb(4 * D * 4, F32, [4, D], at=self.hbuf.lo)
        src = self.x[tile * TT:(tile + 1) * TT, :].rearrange("(a p) d -> p a d", p=128)
        self.dma("pool", xin.ap, src, 1, reads=[], writes=[xin])
        for d in range(16):
            ps = self.next_bank()
            for tc in range(4):
                self.tr(ps.cols(tc * 128, (tc + 1) * 128), xin.idx(tc).cols(d * 128, (d + 1) * 128), self.ident_f)
            self.copy("act" if d % 2 else "dve", self.R.idx(d), ps)

    def final_out(self, tile):
        R = self.R
        xf = self.sb(16 * TT * 4, F32, [16, TT], at=self.hbuf.lo)
        self.rmsnorm(R, 16, ("final_norm",), 1.0 / D, xf)
        for tc in range(4):
            ot = self.otile[tc % 2]
            for dg in range(4):
                ps = self.next_bank()
                for dd in range(4):
                    self.tr(ps.cols(dd * 128, (dd + 1) * 128), xf.idx(dg * 4 + dd).cols(tc * 128, (tc + 1) * 128),
                            self.ident_f)
                self.copy("act" if dg % 2 else "dve", ot.cols(dg * 512, (dg + 1) * 512), ps)
            r0 = tile * TT + tc * 128
            self.dma("pool", self.y[r0:r0 + 128, :], ot.ap, 10 + tc % 2, reads=[ot], writes=[], is_out=True)

    def store_R(self, tile):
        dst = self.rs[:, :, tile * TT:(tile + 1) * TT].rearrange("c p t -> p c t")
        self.dma("pool", dst, self.R.ap, 2, reads=[self.R], writes=[DR(None, ("rs", tile))])

    def load_R(self, tile):
        src = self.rs[:, :, tile * TT:(tile + 1) * TT].rearrange("c p t -> p c t")
        self.dma("pool", self.R.ap, src, 3, reads=[DR(None, ("rs", tile))], writes=[self.R])

    def build(self):
        nc = self.nc
        st = ExitStack()
        self.stack = st
        ntok = self.ntok
        self.x = nc.dram_tensor("x", [ntok, D], F32, kind="ExternalInput").ap()
        self.y = nc.dram_tensor("y", [ntok, D], F32, kind="ExternalOutput").ap()
        vecs_d = nc.dram_tensor("vecs", [128, self.nv], F32, kind="ExternalInput").ap()
        consts_d = nc.dram_tensor("consts", [128, self.ncc], F32, kind="ExternalInput").ap()
        W = {}
        for name, shape in (("mlp_w1", [4, D, DFF]), ("mlp_w2", [4, DFF, D]), ("conv_in", [2, D, 3 * D]),
                            ("conv_out", [2, D, D]), ("attn_in", [2, D, 848]), ("w_uq", [2, 512, 2048]),
                            ("w_uk", [2, 16, 128, 256]), ("w_uv", [2, 16, 256, 128]),
                            ("w_qidx", [2, 512, 1024]), ("attn_out", [2, D, D])):
            if name.startswith("mlp") and not self.layers:
                continue
            if name.startswith("conv") and not any(l % 2 == 0 for l in self.layers):
                continue
            if (name.startswith("w_") or name.startswith("attn")) and not any(l % 2 == 1 for l in self.layers):
                continue
            W[name] = nc.dram_tensor(name, shape, F32, kind="ExternalInput").ap()
        self.W = W
        self.rs = nc.dram_tensor("rs", [16, 128, ntok], F32, kind="Internal").ap()

        self.arena_bytes = 212000
        arena_t = st.enter_context(nc.sbuf_tensor("arena", [128, self.arena_bytes], U8))
        self.arena = arena_t.ap() if hasattr(arena_t, "ap") else arena_t[:]
        psum_t = st.enter_context(nc.psum_tensor("psum", [128, 4096], F32))
        self.psum = psum_t.ap() if hasattr(psum_t, "ap") else psum_t[:]

        self.vecs = self.sb(self.nv * 4, F32)
        self.cst = self.sb(self.ncc * 4, F32)
        self.ident_f = self.cst.cols(self.ccols["ident"], self.ccols["ident"] + 128)
        self.ident_bf = self.sb(256, BF16)
        self.ones_bf = self.sb(256, BF16)
        self.eps_t = self.sb(4, F32)
        self.slots = [self.sb(SLOT_BYTES, BF16) for _ in range(NSLOT)]
        self.R = self.sb(16 * TT * 4, F32, [16, TT])
        self.xn = self.sb(16 * TT * 2, BF16, [16, TT])
        self.hbuf = self.sb(32 * TT * 2, BF16, [32, TT])
        self.sqt = [self.sb(TT * 2, BF16) for _ in range(2)]
        self.rtmp = [self.sb(TT * 2, BF16) for _ in range(2)]
        self.rstd = self.sb(TT * 4, F32)
        self.halo = self.sb(16 * 2 * 4, F32, [16, 2])
        o = self.hbuf.lo + 16 * TT * 2
        self.zt = [self.sb(2112, F32, at=o + i * 2112)for i in range(2)]
        self.zt = [T(z.ap[:, 0:TT + 2], "sb", z.lo, z.hi, 4) for z in self.zt]
        o += 2 * 2112
        self.cgs = [self.sb(TT * 4, F32, at=o + i * TT * 4) for i in range(2)]
        o += 2 * TT * 4
        self.acc = [self.sb(TT * 4, F32, at=o + i * TT * 4) for i in range(2)]
        self.otile = [self.sb(D * 4, F32, at=self.xn.lo + i * D * 4) for i in range(2)]
        self.attn_alloc()

        self.dma("sp", self.vecs.ap, vecs_d, 20, reads=[], writes=[self.vecs])
        self.dma("sp", self.cst.ap, consts_d, 21, reads=[], writes=[self.cst])
        self.copy("dve", self.ident_bf, self.ident_f)
        self.copy("dve", self.ones_bf, self.cst.cols(self.ccols["ones"], self.ccols["ones"] + 128))
        self.P.add("dve", lambda e: e.memset(self.eps_t.ap, EPS), writes=[self.eps_t])

        self.pc = {}
        order = []
        for layer in self.layers:
            j = layer // 2
            per_tile = []
            if layer % 2 == 0:
                for dj in range(16):
                    src = [W["conv_in"][j][:, g * D + dj * 128:g * D + (dj + 1) * 128].rearrange(
                        "(k p) c -> p k c", p=128) for g in range(3)]
                    self.pc[("cin", j, dj)] = self.add_piece(src, [128, 3, 16, 128])
                    per_tile.append(self.pc[("cin", j, dj)])
                for og in range(4):
                    src = W["conv_out"][j][:, og * 512:(og + 1) * 512].rearrange("(k p) c -> p k c", p=128)
                    self.pc[("cout", j, og)] = self.add_piece(src, [128, 16, 512])
                    per_tile.append(self.pc[("cout", j, og)])
            else:
                per_tile += self.attn_pieces(j)
            for hh in range(2):
                for fg in range(hh * 8, hh * 8 + 8):
                    src = W["mlp_w1"][layer][:, fg * 512:(fg + 1) * 512].rearrange("(k p) c -> p k c", p=128)
                    self.pc[("w1", layer, fg)] = self.add_piece(src, [128, 16, 512])
                    per_tile.append(self.pc[("w1", layer, fg)])
                for half in range(2):
                    for kg in range(hh * 4, hh * 4 + 4):
                        src = W["mlp_w2"][layer][kg * 1024:(kg + 1) * 1024,
                                                 half * 1024:(half + 1) * 1024].rearrange("(k p) c -> p k c", p=128)
                        self.pc[("w2", layer, half, kg)] = self.add_piece(src, [128, 8, 1024])
                        per_tile.append(self.pc[("w2", layer, half, kg)])
            order += per_tile * self.ntile
        self.use_order = order
        for op in self.P.lists["pool"]:
            if op.dsem in (100, 101):
                op.seq = self.P.dma_count[op.dsem]
        self.wpos = 0
        self.wissued = 0
        self.whold = None

        nl = len(self.layers)
        for li, layer in enumerate(self.layers):
            for tile in range(self.ntile):
                if li == 0:
                    self.load_x_tile(tile)
                else:
                    self.load_R(tile)
                if layer % 2 == 0:
                    self.conv_mixer(layer, tile % (SEQ // TT) == 0)
                else:
                    self.attn_mixer(layer, tile)
                if self.debug == "attn_only":
                    continue
                self.mlp(layer)
                if li == nl - 1:
                    self.final_out(tile)
                else:
                    self.store_R(tile)
        if nl == 0:
            for tile in range(self.ntile):
                self.load_x_tile(tile)
                self.final_out(tile)
        self.P.emit(st)
        st.close()
        return nc

    def attn_alloc(self):
        if not any(l % 2 == 1 for l in self.layers):
            return
        self.CKVT = self.sb(2 * SEQ * 2, BF16, [2, SEQ])
        self.CKVtok = self.sb(16 * 256 * 2, BF16, [16, 256])
        self.KI = self.sb(SEQ * 2, BF16)
        self.CQ = self.sb(4 * TT * 2, BF16, [4, TT])
        self.MASKT = self.sb(16 * TT * 2, BF16, [16, TT])
        self.thr_rep = self.sb(TT * 4, F32)
        self.vecs_a = self.sb(4 * 8 * 4, F32, [4, 8])
        self.wtok = self.sb(4 * 16 * 4, F32, [4, 16])
        self.wcol = self.sb(16 * 4, F32)
        self.rel = [self.sb(TT * 2, BF16) for _ in range(3)]
        base = (self.sb_off + 63) // 64 * 64
        self.cq_raw = self.sb(4 * TT * 4, F32, [4, TT])
        self.kv_raw = self.sb(2 * TT * 4, F32, [2, TT])
        self.ki_raw = self.sb(TT * 4, F32)
        o = base
        self.junk = self.sb(SEQ * 2, BF16, at=o); o += SEQ * 2
        self.Amat = self.sb(128 * 4, F32, [8, 16], at=o); o += 512
        self.dg = self.sb(128 * 4, F32, at=o); o += 512
        self.tmpdiag = self.sb(TT * 4, F32, at=o); o += TT * 4
        o = base
        self.QH = self.sb(TT * 2, BF16, at=o); o += TT * 2
        self.QL = self.sb(2 * TT * 2, BF16, [2, TT], at=o); o += 2 * TT * 2
        self.E = [self.sb(TT * 2, BF16, at=o + i * TT * 2) for i in range(2)]; o += 2 * TT * 2
        self.Pm = [self.sb(TT * 2, BF16, at=o + i * TT * 2) for i in range(2)]; o += 2 * TT * 2
        self.rden = self.sb(TT * 4, F32, at=o); o += TT * 4
        self.OLn = self.sb(2 * TT * 2, BF16, [2, TT], at=o); o += 2 * TT * 2
        assert o <= self.sb_off
        o = self.hbuf.lo
        self.SC = [self.sb(SEQ * 4, F32, at=o + i * SEQ * 4) for i in range(2)]; o += 2 * SEQ * 4
        self.Qi = [self.sb(128 * 16 * 2, BF16, [128, 16], at=o + i * 4096) for i in range(2)]; o += 8192
        self.Z = [self.sb(16 * 128 * 2, BF16, [16, 128], at=o + i * 4096) for i in range(2)]; o += 8192
        assert o <= self.hbuf.hi

    def attn_pieces(self, j):
        W = self.W
        lst = []

        def reg(key, src, shape):
            self.pc[key] = self.add_piece(src, shape)
            lst.append(self.pc[key])
        reg(("ainA", j), W["attn_in"][j][:, 0:512].rearrange("(k p) c -> p k c", p=128), [128, 16, 512])
        reg(("ainB", j), W["attn_in"][j][:, 512:848].rearrange("(k p) c -> p k c", p=128), [128, 16, 336])
        reg(("wqidx", j), W["w_qidx"][j].rearrange("(k p) c -> p k c", p=128), [128, 4, 1024])
        reg(("wuq", j), W["w_uq"][j].rearrange("(k p) c -> p k c", p=128), [128, 4, 2048])
        reg(("wuk", j), W["w_uk"][j].rearrange("h d c -> d h c"), [128, 16, 256])
        reg(("wuv", j), [W["w_uv"][j][:, cc * 128:(cc + 1) * 128, :].rearrange("h p v -> p h v") for cc in range(2)],
            [128, 2, 16, 128])
        for og in range(4):
            reg(("aout", j, og), W["attn_out"][j][:, og * 512:(og + 1) * 512].rearrange("(k p) c -> p k c", p=128),
                [128, 16, 512])
        return lst

    def rmsnorm_l(self, srcs, gname, inv_n, dsts):
        ps = self.next_bank()
        n = len(srcs)
        for c in range(n):
            sq = self.sqt[c % 2]
            self.act(sq, srcs[c], AF.Square)
            self.mm(ps, self.ones_bf, sq, c == 0, c == n - 1)
        self.act(self.rstd, ps, AF.Ln, scale=inv_n, bias=self.eps_t.ap, extra_reads=[self.eps_t])
        self.act(self.rstd, self.rstd, AF.Exp, scale=-0.5)
        for c in range(n):
            self.stt(dsts[c], srcs[c], self.vcol(gname, c), self.rstd, ALU.mult, ALU.mult, reads=[self.vecs])

    def attn_mixer(self, layer, tile):
        j = layer // 2
        qt = tile % (SEQ // TT)
        t0 = qt * TT
        nkc = 4 * (qt + 1)
        nk = nkc * 128
        R, xn = self.R, self.xn
        cc_ = self.ccols
        ones_f = self.cst.cols(cc_["ones"], cc_["ones"] + 128)
        NIT = 20
        self.rmsnorm(R, 16, ("norm_mix", layer), 1.0 / D, xn)
        slotA = self.wget(self.pc[("ainA", j)])
        for m in range(4):
            ps = self.next_bank()
            for kc in range(16):
                self.mm(ps, slotA.idx(kc).cols(m * 128, (m + 1) * 128), xn.idx(kc), kc == 0, kc == 15)
            self.copy("act" if m % 2 else "dve", self.cq_raw.idx(m), ps)
        slotB = self.wget(self.pc[("ainB", j)])
        for m in range(2):
            ps = self.next_bank()
            for kc in range(16):
                self.mm(ps, slotB.idx(kc).cols(m * 128, (m + 1) * 128), xn.idx(kc), kc == 0, kc == 15)
            self.copy("act" if m % 2 else "dve", self.kv_raw.idx(m), ps)
        ps = self.next_bank()
        for kc in range(16):
            self.mm(ps.parts(0, 64), slotB.idx(kc).cols(256, 320), xn.idx(kc), kc == 0, kc == 15)
        kir = self.ki_raw.parts(0, 64)
        self.copy("act", kir, ps.parts(0, 64))
        ps = self.next_bank()
        for tc in range(4):
            for kc in range(16):
                self.mm(ps.cols(tc * 16, (tc + 1) * 16), xn.idx(kc).cols(tc * 128, (tc + 1) * 128),
                        slotB.idx(kc).cols(320, 336), kc == 0, kc == 15)
        self.act(self.wtok.view(self.wtok.ap.rearrange("p a b -> p (a b)")), ps.cols(0, 64), AF.Identity,
                 scale=1.0 / 32.0)
        self.rmsnorm_l([self.cq_raw.idx(c) for c in range(4)], ("q_norm", j), 1.0 / 512, [self.CQ.idx(c) for c in range(4)])
        kvd = [self.CKVT.idx(c).cols(t0, t0 + TT) for c in range(2)]
        self.rmsnorm_l([self.kv_raw.idx(c) for c in range(2)], ("kv_norm", j), 1.0 / 256, kvd)
        mu = self.cq_raw.idx(0).parts(0, 64)
        var = self.cq_raw.idx(1).parts(0, 64)
        xc = self.cq_raw.idx(2).parts(0, 64)
        sqk = self.cq_raw.idx(3).parts(0, 64)
        on64 = ones_f.parts(0, 64).cols(0, 64)
        self.act(sqk, kir, AF.Square)
        pm = self.next_bank().parts(0, 64)
        self.mm(pm, on64, kir, True, True)
        pv = self.next_bank().parts(0, 64)
        self.mm(pv, on64, sqk, True, True)
        self.act(mu, pm, AF.Identity, scale=1.0 / 64)
        self.tt("dve", var, mu, mu, ALU.mult)
        self.stt(var, pv, 1.0 / 64, var, ALU.mult, ALU.subtract)
        self.act(var, var, AF.Ln, bias=self.eps_t.ap[0:64], extra_reads=[self.eps_t])
        self.act(var, var, AF.Exp, scale=-0.5)
        self.tt("dve", xc, kir, mu, ALU.subtract)
        self.tt("dve", xc, xc, var, ALU.mult)
        kid = self.KI.cols(t0, t0 + TT).parts(0, 64)
        self.ts("dve", kid, xc, self.vcol(("ln_g", j), 0, 1, 64), self.vcol(("ln_b", j), 0, 1, 64), ALU.mult, ALU.add,
                reads=[self.vecs])
        pb = self.next_bank(BF16)
        for i in range(4):
            for c in range(2):
                self.tr(pb.cols(i * 256 + c * 128, i * 256 + (c + 1) * 128),
                        self.CKVT.idx(c).cols(t0 + i * 128, t0 + (i + 1) * 128), self.ident_bf)
        dstk = self.CKVtok.view(self.CKVtok.ap[:, 4 * qt:4 * qt + 4, :].rearrange("p a b -> p (a b)"))
        dstk.lo = self.CKVtok.lo + 4 * qt * 512
        dstk.hi = dstk.lo + 4 * 512
        self.copy("dve", dstk, pb)
        slotQ = self.wget(self.pc[("wqidx", j)])
        for st in range(4):
            Qi, Z, SC = self.Qi[st % 2], self.Z[st % 2], self.SC[st % 2]
            va = self.vecs_a.idx(st)
            v_hi, v_lo, v_w0, v_mid, v_cnt, v_gew, v_t = [va.cols(i, i + 1) for i in range(7)]
            for hg in range(4):
                ps = self.bank(6 + hg % 2)
                for hh in range(4):
                    h = hg * 4 + hh
                    for kc in range(4):
                        self.mm(ps.parts(0, 64).cols(hh * 128, (hh + 1) * 128), slotQ.idx(kc).cols(h * 64, (h + 1) * 64),
                                self.CQ.idx(kc).cols(st * 128, (st + 1) * 128), kc == 0, kc == 3)
                dq = Qi.view(Qi.ap[0:64, :, hg * 4:hg * 4 + 4].rearrange("p t h -> p h t"))
                sq_ = ps.view(ps.ap[0:64, :].rearrange("p (h t) -> p h t", t=128))
                self.copy("act" if hg % 2 else "dve", dq, sq_)
            wt = self.wtok.idx(st)
            for tp in range(8):
                self.ts("dve", self.Amat.idx(tp), wt, self.cst.ap[:, cc_["m8"] + tp:cc_["m8"] + tp + 1], None, ALU.mult,
                        reads=[self.cst])
            pw = self.bank(6).cols(0, 16)
            self.mm(pw, self.Amat.view(self.Amat.ap.rearrange("p a b -> p (a b)")),
                    self.cst.cols(cc_["bsel"], cc_["bsel"] + 16), True, True)
            self.copy("dve", self.wcol, pw)
            self.P.add("pool", lambda e, Z=Z: e.memset(Z.ap, 0.0), writes=[Z])
            d8 = self.cst.cols(cc_["d8"], cc_["d8"] + 8)
            for g in range(16):
                zg = Z.idx(g).cols(8 * g, 8 * g + 8)
                self.ts("dve", zg, d8, self.wcol.ap[:, g:g + 1], None, ALU.mult, reads=[self.wcol])
            for sb_ in range(qt + 1):
                scb = self.bank(4 + (st * 4 + sb_) % 2)
                kis = self.KI.cols(sb_ * TT, (sb_ + 1) * TT).parts(0, 64)

                def s1(g):
                    lq = Qi.view(Qi.ap[0:64, 8 * g:8 * g + 8, :].rearrange("p t h -> p (t h)"))
                    self.mm(self.bank(g % 4), lq, kis, True, True)
                s1(0)
                for g in range(16):
                    if g + 1 < 16:
                        s1(g + 1)
                    rel = self.rel[g % 3]
                    self.act(rel, self.bank(g % 4), AF.Relu)
                    self.mm(scb, Z.idx(g), rel, g == 0, g == 15)
                scd = SC.cols(sb_ * TT, (sb_ + 1) * TT)
                if sb_ == qt:
                    self.tt("dve", scd, scb, self.cst.cols(cc_[("cb", st)], cc_[("cb", st)] + TT), ALU.add)
                else:
                    self.copy("act", scd, scb)
            if qt == 0 and st < 2:
                self.P.add("dve", lambda e, v=v_lo: e.memset(v.ap, -1.0e29), writes=[v_lo])
            else:
                scv = SC.cols(0, nk)
                self.P.add("dve", lambda e, o=v_hi, i=scv: e.tensor_reduce(o.ap, i.ap, AX.X, ALU.max),
                           reads=[scv], writes=[v_hi])
                cbt = self.cst.cols(cc_[("cb", st)], cc_[("cb", st)] + TT)
                self.stt(self.tmpdiag, cbt, -2.0, SC.cols(qt * TT, nk), ALU.mult, ALU.add)
                self.P.add("dve", lambda e, o=v_lo, i=self.tmpdiag: e.tensor_reduce(o.ap, i.ap, AX.X, ALU.min),
                           reads=[self.tmpdiag], writes=[v_lo])
                if qt > 0:
                    scf = SC.cols(0, qt * TT)
                    self.P.add("dve", lambda e, o=v_t, i=scf: e.tensor_reduce(o.ap, i.ap, AX.X, ALU.min),
                               reads=[scf], writes=[v_t])
                    self.tt("dve", v_lo, v_lo, v_t, ALU.min)
                self.tt("dve", v_w0, v_hi, v_lo, ALU.subtract)
                jk = self.junk.cols(0, nk)
                for k in range(NIT):
                    f = 2.0 ** -(k + 1)
                    self.stt(v_mid, v_w0, f, v_lo, ALU.mult, ALU.add)
                    self.P.add("dve", lambda e, o=jk, i=scv, m=v_mid, c=v_cnt: e.tensor_scalar(
                        o.ap, i.ap, m.ap, None, ALU.is_ge, op1=ALU.add, accum_out=c.ap),
                        reads=[scv, v_mid, jk], writes=[v_cnt])
                    self.ts("dve", v_gew, v_cnt, 255.5, f, ALU.is_ge, ALU.mult)
                    self.stt(v_lo, v_gew, v_w0.ap, v_lo, ALU.mult, ALU.add, reads=[v_w0])
            self.ts("dve", self.dg, self.ident_f, v_lo.ap, None, ALU.mult, reads=[v_lo])
            pt = self.bank(7)
            for r in range(4):
                self.mm(pt.cols(r * 128, (r + 1) * 128), ones_f, self.dg, True, True)
            self.copy("act", self.thr_rep, pt)
            for scg in range(qt + 1):
                pT = self.bank(6 + scg % 2)
                for i in range(4):
                    sc = scg * 4 + i
                    self.tr(pT.cols(i * 128, (i + 1) * 128), SC.cols(sc * 128, (sc + 1) * 128), self.ident_f)
                mo = self.MASKT.view(self.MASKT.ap[:, scg * 4:scg * 4 + 4, st * 128:(st + 1) * 128])
                mo.lo = self.MASKT.lo + scg * 4 * TT * 2
                mo.hi = mo.lo + 4 * TT * 2
                pin = pT.view(pT.ap.rearrange("p (a b) -> p a b", b=128))
                tin = self.thr_rep.view(self.thr_rep.ap.rearrange("p (a b) -> p a b", b=128))
                self.tt("dve", mo, pin, tin, ALU.is_ge)
        sUQ = self.wget(self.pc[("wuq", j)], hold=True)
        sUK = self.wget(self.pc[("wuk", j)])
        sUV = self.wget(self.pc[("wuv", j)])
        O = xn
        scale = 128.0 ** -0.5
        for h in range(16):
            b6 = self.bank(6)
            for kc in range(4):
                self.mm(b6, sUQ.idx(kc).cols(h * 128, (h + 1) * 128), self.CQ.idx(kc), kc == 0, kc == 3)
            self.copy("act", self.QH, b6)
            for c in range(2):
                bq = self.bank(7 - c)
                self.mm(bq, sUK.idx(h).cols(c * 128, (c + 1) * 128), self.QH, True, True)
                self.act(self.QL.idx(c), bq, AF.Identity, scale=scale)

            def qk(sc):
                bs = self.bank(3 + sc % 3)
                for c in range(2):
                    self.mm(bs, self.CKVT.idx(c).cols(sc * 128, (sc + 1) * 128), self.QL.idx(c), c == 0, c == 1)
            qk(0)
            for sc in range(nkc):
                if sc + 1 < nkc:
                    qk(sc + 1)
                E, Pm = self.E[sc % 2], self.Pm[sc % 2]
                self.act(E, self.bank(3 + sc % 3), AF.Exp)
                self.tt("pool" if sc % 2 else "dve", Pm, E, self.MASKT.idx(sc), ALU.mult)
                for c in range(2):
                    self.mm(self.bank(c), self.CKVtok.idx(sc).cols(c * 128, (c + 1) * 128), Pm, sc == 0, sc == nkc - 1)
                self.mm(self.bank(2), self.ones_bf, Pm, sc == 0, sc == nkc - 1)
            self.P.add("dve", lambda e, o=self.rden, i=self.bank(2): e.reciprocal(o.ap, i.ap),
                       reads=[self.bank(2)], writes=[self.rden])
            for c in range(2):
                self.tt("dve", self.OLn.idx(c), self.bank(c), self.rden, ALU.mult)
            bo = self.bank(6)
            for c in range(2):
                self.mm(bo, sUV.view(sUV.ap[:, c, h, :]), self.OLn.idx(c), c == 0, c == 1)
            self.copy("act", O.idx(h), bo)
        self.ps_rr = 3
        self.whold = None
        for og in range(4):
            slot = self.wget(self.pc[("aout", j, og)])
            for dd in range(4):
                d = og * 4 + dd
                ps = self.next_bank()
                for kc in range(16):
                    self.mm(ps, slot.idx(kc).cols(dd * 128, (dd + 1) * 128), O.idx(kc), kc == 0, kc == 15)
                self.tt("dve", R.idx(d), ps, R.idx(d), ALU.add)


_CACHE = {}


def get_prog(nseq, layers):
    key = (nseq, tuple(layers))
    if key not in _CACHE:
        b = Builder(nseq=nseq, layers=layers)
        _CACHE[key] = b.build()
    return _CACHE[key]


def make_inputs(inputs, nseq, ncores):
    x = np.ascontiguousarray(np.asarray(inputs["x"], np.float32))
    shared = {"vecs": build_vecs(inputs), "consts": build_consts()}
    for name in ("mlp_w1", "mlp_w2", "conv_in", "conv_out", "attn_in", "attn_out"):
        shared[name] = np.ascontiguousarray(np.asarray(inputs[name], np.float32))
    shared["w_uq"] = np.ascontiguousarray(np.asarray(inputs["w_uq"], np.float32)).reshape(2, 512, 2048)
    shared["w_qidx"] = np.ascontiguousarray(np.asarray(inputs["w_qidx"], np.float32)).reshape(2, 512, 1024)
    shared["w_uk"] = np.ascontiguousarray(np.asarray(inputs["w_uk"], np.float32))
    shared["w_uv"] = np.ascontiguousarray(np.asarray(inputs["w_uv"], np.float32))
    in_maps = []
    for c in range(ncores):
        m = dict(shared)
        m["x"] = x[c * nseq:(c + 1) * nseq].reshape(nseq * SEQ, D)
        in_maps.append(m)
    return in_maps


def run(inputs, nseq=2, layers=(0, 1, 2, 3), ncores=NCORES):
    nc = get_prog(nseq, layers)
    in_maps = make_inputs(inputs, nseq, ncores)
    if not layers:
        in_maps = [{k: v for k, v in m.items() if k in ("x", "vecs", "consts")} for m in in_maps]
    elif not any(l % 2 == 1 for l in layers):
        in_maps = [{k: v for k, v in m.items() if not (k.startswith("w_") or k.startswith("attn"))} for m in in_maps]
    res = run_bass_kernel_spmd(nc, in_maps, core_ids=list(range(ncores)))
    out = np.stack([np.asarray(r["y"]).reshape(nseq, SEQ, D) for r in res.results], 0)
    return out.reshape(ncores * nseq, SEQ, D).astype(np.float32)


def kernel(**inputs):
    return run(inputs)
```

```python
from contextlib import ExitStack
import numpy as np
import concourse.bass as bass
import concourse.mybir as mybir
from concourse.bass_utils import run_bass_kernel_spmd

F32 = mybir.dt.float32
BF16 = mybir.dt.bfloat16
U8 = mybir.dt.uint8
ALU = mybir.AluOpType
AF = mybir.ActivationFunctionType
AX = mybir.AxisListType

D = 2048
DFF = 8192
SEQ = 2048
NCORES = 8
TT = 512
EPS = 1e-6
NEG = -1.0e30
SLOT_BYTES = 16384
NSLOT = 3
CELL = 256
SAME_ENG_SYNC = True
ESZ = {F32: 4, BF16: 2, U8: 1}


class T:
    __slots__ = ("ap", "space", "lo", "hi", "esz")

    def __init__(self, ap, space, lo, hi, esz):
        self.ap, self.space, self.lo, self.hi, self.esz = ap, space, lo, hi, esz

    def idx(self, i):
        n = self.ap.shape[1]
        sz = (self.hi - self.lo) // n
        return T(self.ap[:, i], self.space, self.lo + i * sz, self.lo + (i + 1) * sz, self.esz)

    def cols(self, a, b):
        assert len(self.ap.shape) == 2
        return T(self.ap[:, a:b], self.space, self.lo + a * self.esz, self.lo + b * self.esz, self.esz)

    def parts(self, a, b):
        return T(self.ap[a:b], self.space, self.lo, self.hi, self.esz)

    def view(self, ap):
        return T(ap, self.space, self.lo, self.hi, self.esz)


class DR:
    __slots__ = ("ap", "key")

    def __init__(self, ap, key):
        self.ap, self.key = ap, key


class Op:
    __slots__ = ("eng", "fn", "deps", "key", "seq", "sig", "val", "dsem")


class Prog:
    ENGS = ("pe", "act", "dve", "pool", "sp")

    def __init__(self, nc):
        self.nc = nc
        self.lists = {e: [] for e in self.ENGS}
        self.cells = {}
        self.nseq = {e: 0 for e in self.ENGS}
        self.dma_count = {}
        self.out_dma = {}

    def _cells(self, t):
        if isinstance(t, DR):
            return [("dram", t.key)]
        if t.space == "ps":
            return [("ps", c) for c in range(t.lo // 2048, (t.hi + 2047) // 2048)]
        return [("sb", c) for c in range(t.lo // CELL, (t.hi + CELL - 1) // CELL)]

    def add(self, eng, fn, reads=(), writes=(), dsem=None, is_out=False):
        op = Op()
        op.eng, op.fn, op.sig, op.val, op.dsem = eng, fn, False, 0, dsem
        if dsem is not None:
            op.key = ("dma", dsem)
            self.dma_count[dsem] = self.dma_count.get(dsem, 0) + 16
            op.seq = self.dma_count[dsem]
            if is_out:
                self.out_dma[dsem] = op.seq
        else:
            op.key = eng
            self.nseq[eng] += 1
            op.seq = self.nseq[eng]
        deps = {}

        def need(o):
            if o is None or o is op:
                return
            if o.dsem is None and op.dsem is None and o.eng == eng:
                if eng == "pe" or not SAME_ENG_SYNC:
                    return
            cur = deps.get(o.key)
            if cur is None or cur.seq < o.seq:
                deps[o.key] = o

        for t in reads:
            for c in self._cells(t):
                st = self.cells.get(c)
                if st is None:
                    st = self.cells[c] = [None, {}]
                need(st[0])
                cur = st[1].get(op.key)
                if cur is None or cur.seq < op.seq:
                    st[1][op.key] = op
        for t in writes:
            for c in self._cells(t):
                st = self.cells.get(c)
                if st is None:
                    st = self.cells[c] = [None, {}]
                need(st[0])
                for r in st[1].values():
                    need(r)
                st[0] = op
                st[1] = {}
        op.deps = list(deps.values())
        for o in op.deps:
            if o.dsem is None:
                o.sig = True
        self.lists[eng].append(op)
        return op

    def emit(self, stack):
        nc = self.nc
        sems = {e: stack.enter_context(nc.semaphore("sem_" + e)) for e in self.ENGS}
        dsems = {k: stack.enter_context(nc.semaphore("dsem_%s" % str(k))) for k in self.dma_count}
        for e in self.ENGS:
            cnt = 0
            for op in self.lists[e]:
                if op.dsem is None and op.sig:
                    cnt += 1
                    op.val = cnt
        block = stack.enter_context(nc.Block())

        def run(ename, eh):
            known = {}
            for op in self.lists[ename]:
                for o in op.deps:
                    if o.dsem is None:
                        s, v = sems[o.eng], o.val
                    else:
                        s, v = dsems[o.dsem], o.seq
                    if known.get(id(s), 0) >= v:
                        continue
                    known[id(s)] = v
                    eh.wait_ge(s, v)
                ins = op.fn(eh)
                if op.dsem is not None:
                    ins.then_inc(dsems[op.dsem], 16)
                elif op.sig:
                    ins.then_inc(sems[ename], 1)
            if ename == "sp":
                for k, v in self.out_dma.items():
                    eh.wait_ge(dsems[k], v)

        @block.tensor
        def _(e):
            run("pe", e)

        @block.scalar
        def _(e):
            run("act", e)

        @block.vector
        def _(e):
            run("dve", e)

        @block.gpsimd
        def _(e):
            run("pool", e)

        @block.sync
        def _(e):
            run("sp", e)


def vec_layout():
    cols = {}
    n = 0

    def put(name, k):
        nonlocal n
        cols[name] = n
        n += k
    for i in range(4):
        put(("norm_mix", i), 16)
        put(("norm_mlp", i), 16)
    put(("final_norm",), 16)
    for j in range(2):
        for k in range(3):
            put(("conv_w", j, k), 16)
        put(("q_norm", j), 4)
        put(("kv_norm", j), 2)
        put(("ln_g", j), 1)
        put(("ln_b", j), 1)
    return cols, n


def const_layout():
    cols = {}
    n = 0

    def put(name, k):
        nonlocal n
        cols[name] = n
        n += k
    put("ident", 128)
    put("ones", 128)
    for s in range(4):
        put(("cb", s), 512)
    put("m8", 8)
    put("d8", 8)
    put("bsel", 16)
    return cols, n


def build_vecs(inp):
    cols, n = vec_layout()
    v = np.zeros((128, n), np.float32)

    def setv(name, arr):
        arr = np.asarray(arr, np.float32).reshape(-1)
        k = arr.size // 128
        if k == 0:
            v[:arr.size, cols[name]] = arr
        else:
            v[:, cols[name]:cols[name] + k] = arr.reshape(k, 128).T
    for i in range(4):
        setv(("norm_mix", i), inp["norm_mix"][i])
        setv(("norm_mlp", i), inp["norm_mlp"][i])
    setv(("final_norm",), inp["final_norm"])
    for j in range(2):
        for k in range(3):
            setv(("conv_w", j, k), inp["conv_w"][j, k])
        setv(("q_norm", j), inp["q_norm"][j])
        setv(("kv_norm", j), inp["kv_norm"][j])
        setv(("ln_g", j), inp["kidx_ln_g"][j])
        setv(("ln_b", j), inp["kidx_ln_b"][j])
    return v


def build_consts():
    cols, n = const_layout()
    c = np.zeros((128, n), np.float32)
    p = np.arange(128)
    c[:, cols["ident"]:cols["ident"] + 128] = np.eye(128, dtype=np.float32)
    c[:, cols["ones"]:cols["ones"] + 128] = 1.0
    s = np.arange(512)
    for st in range(4):
        vis = s[None, :] <= (st * 128 + p)[:, None]
        c[:, cols[("cb", st)]:cols[("cb", st)] + 512] = np.where(vis, 0.0, NEG)
    c[:, cols["m8"]:cols["m8"] + 8] = (p[:, None] % 8 == np.arange(8)[None, :])
    c[:, cols["d8"]:cols["d8"] + 8] = (p[:, None] // 16 == np.arange(8)[None, :])
    c[:, cols["bsel"]:cols["bsel"] + 16] = (p[:, None] // 8 == np.arange(16)[None, :])
    return c


class Builder:
    def __init__(self, nseq=2, layers=(0, 1, 2, 3), do_final=True, debug=None, ntile=None):
        self.nseq = nseq
        self.ntok = nseq * SEQ
        self.ntile = ntile or self.ntok // TT
        self.layers = tuple(layers)
        self.do_final = do_final
        self.debug = debug
        self.nc = bass.Bass("TRN2", target_bir_lowering=False)
        self.P = Prog(self.nc)
        self.vcols, self.nv = vec_layout()
        self.ccols, self.ncc = const_layout()
        self.sb_off = 0
        self.ps_rr = 0
        self.pieces = []
        self.use_order = []
        self.dsem_n = 0

    def new_dsem(self):
        self.dsem_n += 1
        return self.dsem_n

    def sb(self, nbytes, dtype, shape=None, at=None):
        esz = ESZ[dtype]
        if at is None:
            lo = (self.sb_off + CELL - 1) // CELL * CELL
            self.sb_off = lo + nbytes
            assert self.sb_off <= self.arena_bytes, ("SBUF overflow", self.sb_off)
        else:
            lo = at
        ap = self.arena[:, lo:lo + nbytes].bitcast(dtype)
        if shape is not None and len(shape) == 2:
            ap = ap.rearrange("p (a b) -> p a b", b=shape[1])
        elif shape is not None and len(shape) == 3:
            ap = ap.rearrange("p (a b c) -> p a b c", b=shape[1], c=shape[2])
        return T(ap, "sb", lo, lo + nbytes, esz)

    def bank(self, i, dtype=F32):
        ap = self.psum[:, i * 512:(i + 1) * 512]
        if dtype != F32:
            ap = ap.bitcast(dtype)
        return T(ap, "ps", i * 2048, (i + 1) * 2048, ESZ[dtype])

    def next_bank(self, dtype=F32):
        b = self.bank(self.ps_rr % 8, dtype)
        self.ps_rr += 1
        return b

    def mm(self, out, lhsT, rhs, start, stop):
        self.P.add("pe", lambda e: e.matmul(out.ap, lhsT.ap, rhs.ap, start=start, stop=stop),
                   reads=[lhsT, rhs], writes=[out])

    def tr(self, out, in_, ident):
        self.P.add("pe", lambda e: e.transpose(out.ap, in_.ap, ident.ap), reads=[in_, ident], writes=[out])

    def act(self, out, in_, func, scale=1.0, bias=0.0, extra_reads=()):
        self.P.add("act", lambda e: e.activation(out.ap, in_.ap, func, bias=bias, scale=scale),
                   reads=[in_] + list(extra_reads), writes=[out])

    def tt(self, eng, out, a, b, op):
        h = "dve" if eng == "dve" else "pool"
        self.P.add(h, lambda e: e.tensor_tensor(out.ap, a.ap, b.ap, op), reads=[a, b], writes=[out])

    def ts(self, eng, out, a, s1, s2, op0, op1=None, reads=(), accum=None):
        kw = {}
        if op1 is not None:
            kw["op1"] = op1
        w = [out]
        if accum is not None:
            kw["accum_out"] = accum.ap
            w.append(accum)
        self.P.add(eng, lambda e: e.tensor_scalar(out.ap, a.ap, s1, s2, op0, **kw),
                   reads=[a] + list(reads), writes=w)

    def stt(self, out, a, scalar, b, op0, op1, reads=()):
        self.P.add("dve", lambda e: e.scalar_tensor_tensor(out.ap, a.ap, scalar, b.ap, op0, op1),
                   reads=[a, b] + list(reads), writes=[out])

    def copy(self, eng, out, in_):
        if eng == "act":
            self.P.add("act", lambda e: e.copy(out.ap, in_.ap), reads=[in_], writes=[out])
        else:
            self.P.add(eng, lambda e: e.tensor_copy(out.ap, in_.ap), reads=[in_], writes=[out])

    def dma(self, q, out, in_, dsem, reads, writes, is_out=False):
        self.P.add(q, lambda e: e.dma_start(out=out, in_=in_), reads=reads, writes=writes, dsem=dsem,
                   is_out=is_out)

    def vcol(self, name, c=0, n=1, parts=128):
        o = self.vcols[name] + c
        return self.vecs.ap[0:parts, o:o + n]

    def add_piece(self, src_ap, shape):
        n = int(np.prod(shape[1:]))
        assert n * 2 <= SLOT_BYTES
        idx = len(self.pieces)
        scr = self.nc.dram_tensor("wscr%d" % idx, [128, n], BF16, kind="Internal").ap()
        res = DR(scr, ("w", idx))
        sem = 100 + 2 * self.cast_group + (idx % 2)
        if len(shape) == 3:
            self.dma("pool", scr.rearrange("p (a b) -> p a b", b=shape[2]), src_ap, sem, reads=[], writes=[res])
        else:
            sub = shape[2] * shape[3]
            for g in range(shape[1]):
                dst = scr[:, g * sub:(g + 1) * sub].rearrange("p (a b) -> p a b", b=shape[3])
                self.dma("pool", dst, src_ap[g], sem, reads=[], writes=[DR(None, ("w", idx, g))])
        self.pieces.append((res, n, shape))
        return idx

    def wget(self, idx, hold=False):
        pos = self.wpos
        assert self.use_order[pos] == idx, (pos, idx, self.use_order[pos])
        if hold and self.whold is None:
            self.whold = pos
        base = pos if self.whold is None else self.whold
        while self.wissued < min(len(self.use_order), base + NSLOT):
            k = self.wissued
            res, n, shape = self.pieces[self.use_order[k]]
            s = k % NSLOT
            st = self.slots[s]
            dst = st.ap[:, 0:n]
            rr = [res] if len(shape) == 3 else [DR(None, ("w", self.use_order[k], g)) for g in range(shape[1])]
            self.dma("sp", dst, res.ap, 200 + s, reads=rr, writes=[T(dst, "sb", st.lo, st.lo + 2 * n, 2)])
            self.wissued += 1
        self.wpos += 1
        res, n, shape = self.pieces[idx]
        st = self.slots[pos % NSLOT]
        ap = st.ap[:, 0:n]
        if len(shape) == 3:
            ap = ap.rearrange("p (a b) -> p a b", b=shape[2])
        elif len(shape) == 4:
            ap = ap.rearrange("p (a b c) -> p a b c", b=shape[2], c=shape[3])
        return T(ap, "sb", st.lo, st.lo + 2 * n, 2)

    def rmsnorm(self, src, nch, gname, inv_n, dst, ones_parts=128, width=TT):
        ps = self.next_bank()
        for c in range(nch):
            sq = self.sqt[c % 2]
            self.act(sq, src.idx(c), AF.Square)
            self.mm(ps, self.ones_bf, sq, c == 0, c == nch - 1)
        self.act(self.rstd, ps, AF.Ln, scale=inv_n, bias=self.eps_t.ap, extra_reads=[self.eps_t])
        self.act(self.rstd, self.rstd, AF.Exp, scale=-0.5)
        for c in range(nch):
            self.stt(dst.idx(c), src.idx(c), self.vcol(gname, c), self.rstd, ALU.mult, ALU.mult,
                     reads=[self.vecs])

    def mlp(self, layer):
        R, xn, h = self.R, self.xn, self.hbuf
        self.rmsnorm(R, 16, ("norm_mlp", layer), 1.0 / D, xn)
        for hh in range(2):
            for fg in range(hh * 8, hh * 8 + 8):
                slot = self.wget(self.pc[("w1", layer, fg)])
                for jj in range(4):
                    j = (fg - hh * 8) * 4 + jj
                    ps = self.next_bank()
                    for kc in range(16):
                        self.mm(ps, slot.idx(kc).cols(jj * 128, (jj + 1) * 128), xn.idx(kc), kc == 0, kc == 15)
                    tmp = self.rtmp[j % 2]
                    self.act(tmp, ps, AF.Relu)
                    self.tt("dve", h.idx(j), tmp, tmp, ALU.mult)
            for half in range(2):
                banks = [self.bank(i) for i in range(8)]
                for kg in range(hh * 4, hh * 4 + 4):
                    slot = self.wget(self.pc[("w2", layer, half, kg)])
                    for kk in range(8):
                        k = (kg - hh * 4) * 8 + kk
                        for dl in range(8):
                            self.mm(banks[dl], slot.idx(kk).cols(dl * 128, (dl + 1) * 128), h.idx(k), k == 0, k == 31)
                for dl in range(8):
                    d = half * 8 + dl
                    self.tt("dve", R.idx(d), banks[dl], R.idx(d), ALU.add)
        self.ps_rr = 0

    def conv_mixer(self, layer, first_in_seq):
        j = layer // 2
        R, xn = self.R, self.xn
        u = self.sb(16 * TT * 2, BF16, [16, TT], at=self.hbuf.lo)
        self.rmsnorm(R, 16, ("norm_mix", layer), 1.0 / D, xn)
        for dj in range(16):
            slot = self.wget(self.pc[("cin", j, dj)])
            bks = [self.next_bank() for _ in range(3)]
            for g in range(3):
                for kc in range(16):
                    lw = slot.view(slot.ap[:, g, kc, :])
                    self.mm(bks[g], lw, xn.idx(kc), kc == 0, kc == 15)
            z = self.zt[dj % 2]
            cgs = self.cgs[dj % 2]
            acc = self.acc[dj % 2]
            halo = self.halo.idx(dj)
            self.copy("act", cgs, bks[1])
            if first_in_seq:
                self.P.add("pool", lambda e, z=z: e.memset(z.ap[:, 0:2], 0.0), writes=[z.cols(0, 2)])
            else:
                self.copy("pool", z.cols(0, 2), halo)
            self.tt("dve", z.cols(2, TT + 2), bks[2], cgs, ALU.mult)
            self.ts("dve", acc, z.cols(2, TT + 2), self.vcol(("conv_w", j, 2), dj), None, ALU.mult,
                    reads=[self.vecs])
            self.stt(acc, z.cols(1, TT + 1), self.vcol(("conv_w", j, 1), dj), acc, ALU.mult, ALU.add,
                     reads=[self.vecs])
            self.stt(acc, z.cols(0, TT), self.vcol(("conv_w", j, 0), dj), acc, ALU.mult, ALU.add,
                     reads=[self.vecs])
            self.copy("pool", halo, z.cols(TT, TT + 2))
            self.tt("dve", u.idx(dj), bks[0], acc, ALU.mult)
        for og in range(4):
            slot = self.wget(self.pc[("cout", j, og)])
            for dd in range(4):
                d = og * 4 + dd
                ps = self.next_bank()
                for kc in range(16):
                    self.mm(ps, slot.idx(kc).cols(dd * 128, (dd + 1) * 128), u.idx(kc), kc == 0, kc == 15)
                self.tt("dve", R.idx(d), ps, R.idx(d), ALU.add)

    def load_x_tile(self, tile):
        xin = self.sb(4 * D * 4, F32, [4, D], at=self.hbuf.lo)
        src = self.x[tile * TT:(tile + 1) * TT, :].rearrange("(a p) d -> p a d", p=128)
        self.dma("sp", xin.ap, src, 1, reads=[], writes=[xin])
        for d in range(16):
            ps = self.next_bank()
            for tc in range(4):
                self.tr(ps.cols(tc * 128, (tc + 1) * 128), xin.idx(tc).cols(d * 128, (d + 1) * 128), self.ident_f)
            self.copy("act" if d % 2 else "dve", self.R.idx(d), ps)

    def final_out(self, tile):
        R = self.R
        xf = self.sb(16 * TT * 4, F32, [16, TT], at=self.hbuf.lo)
        self.rmsnorm(R, 16, ("final_norm",), 1.0 / D, xf)
        for tc in range(4):
            ot = self.otile[tc % 2]
            for dg in range(4):
                ps = self.next_bank()
                for dd in range(4):
                    self.tr(ps.cols(dd * 128, (dd + 1) * 128), xf.idx(dg * 4 + dd).cols(tc * 128, (tc + 1) * 128),
                            self.ident_f)
                self.copy("act" if dg % 2 else "dve", ot.cols(dg * 512, (dg + 1) * 512), ps)
            r0 = tile * TT + tc * 128
            self.dma("sp", self.y[r0:r0 + 128, :], ot.ap, 10 + tc % 2, reads=[ot], writes=[], is_out=True)

    def store_R(self, tile):
        dst = self.rs[:, :, tile * TT:(tile + 1) * TT].rearrange("c p t -> p c t")
        self.dma("sp", dst, self.R.ap, 2, reads=[self.R], writes=[DR(None, ("rs", tile))])

    def load_R(self, tile):
        src = self.rs[:, :, tile * TT:(tile + 1) * TT].rearrange("c p t -> p c t")
        self.dma("sp", self.R.ap, src, 3, reads=[DR(None, ("rs", tile))], writes=[self.R])

    def build(self):
        nc = self.nc
        st = ExitStack()
        self.stack = st
        ntok = self.ntok
        self.x = nc.dram_tensor("x", [ntok, D], F32, kind="ExternalInput").ap()
        self.y = nc.dram_tensor("y", [ntok, D], F32, kind="ExternalOutput").ap()
        vecs_d = nc.dram_tensor("vecs", [128, self.nv], F32, kind="ExternalInput").ap()
        consts_d = nc.dram_tensor("consts", [128, self.ncc], F32, kind="ExternalInput").ap()
        W = {}
        for name, shape in (("mlp_w1", [4, D, DFF]), ("mlp_w2", [4, DFF, D]), ("conv_in", [2, D, 3 * D]),
                            ("conv_out", [2, D, D]), ("attn_in", [2, D, 848]), ("w_uq", [2, 512, 2048]),
                            ("w_uk", [2, 16, 128, 256]), ("w_uv", [2, 16, 256, 128]),
                            ("w_qidx", [2, 512, 1024]), ("attn_out", [2, D, D])):
            if name.startswith("mlp") and not self.layers:
                continue
            if name.startswith("conv") and not any(l % 2 == 0 for l in self.layers):
                continue
            if (name.startswith("w_") or name.startswith("attn")) and not any(l % 2 == 1 for l in self.layers):
                continue
            W[name] = nc.dram_tensor(name, shape, F32, kind="ExternalInput").ap()
        self.W = W
        self.rs = nc.dram_tensor("rs", [16, 128, ntok], F32, kind="Internal").ap()

        self.arena_bytes = 212000
        arena_t = st.enter_context(nc.sbuf_tensor("arena", [128, self.arena_bytes], U8))
        self.arena = arena_t.ap() if hasattr(arena_t, "ap") else arena_t[:]
        psum_t = st.enter_context(nc.psum_tensor("psum", [128, 4096], F32))
        self.psum = psum_t.ap() if hasattr(psum_t, "ap") else psum_t[:]

        self.vecs = self.sb(self.nv * 4, F32)
        self.cst = self.sb(self.ncc * 4, F32)
        self.ident_f = self.cst.cols(self.ccols["ident"], self.ccols["ident"] + 128)
        self.ident_bf = self.sb(256, BF16)
        self.ones_bf = self.sb(256, BF16)
        self.eps_t = self.sb(4, F32)
        self.slots = [self.sb(SLOT_BYTES, BF16) for _ in range(NSLOT)]
        self.R = self.sb(16 * TT * 4, F32, [16, TT])
        self.xn = self.sb(16 * TT * 2, BF16, [16, TT])
        self.hbuf = self.sb(32 * TT * 2, BF16, [32, TT])
        self.sqt = [self.sb(TT * 2, BF16) for _ in range(2)]
        self.rtmp = [self.sb(TT * 2, BF16) for _ in range(2)]
        self.rstd = self.sb(TT * 4, F32)
        self.halo = self.sb(16 * 2 * 4, F32, [16, 2])
        o = self.hbuf.lo + 16 * TT * 2
        self.zt = [self.sb(2112, F32, at=o + i * 2112)for i in range(2)]
        self.zt = [T(z.ap[:, 0:TT + 2], "sb", z.lo, z.hi, 4) for z in self.zt]
        o += 2 * 2112
        self.cgs = [self.sb(TT * 4, F32, at=o + i * TT * 4) for i in range(2)]
        o += 2 * TT * 4
        self.acc = [self.sb(TT * 4, F32, at=o + i * TT * 4) for i in range(2)]
        self.otile = [self.sb(D * 4, F32, at=self.xn.lo + i * D * 4) for i in range(2)]
        self.attn_alloc()

        self.dma("sp", self.vecs.ap, vecs_d, 20, reads=[], writes=[self.vecs])
        self.dma("sp", self.cst.ap, consts_d, 21, reads=[], writes=[self.cst])
        self.copy("dve", self.ident_bf, self.ident_f)
        self.copy("dve", self.ones_bf, self.cst.cols(self.ccols["ones"], self.ccols["ones"] + 128))
        self.P.add("dve", lambda e: e.memset(self.eps_t.ap, EPS), writes=[self.eps_t])

        self.pc = {}
        order = []
        cast_marks = []
        for layer in self.layers:
            j = layer // 2
            per_tile = []
            n0 = len(self.P.lists["pool"])
            self.cast_group = len(cast_marks)
            if layer % 2 == 0:
                for dj in range(16):
                    src = [W["conv_in"][j][:, g * D + dj * 128:g * D + (dj + 1) * 128].rearrange(
                        "(k p) c -> p k c", p=128) for g in range(3)]
                    self.pc[("cin", j, dj)] = self.add_piece(src, [128, 3, 16, 128])
                    per_tile.append(self.pc[("cin", j, dj)])
                for og in range(4):
                    src = W["conv_out"][j][:, og * 512:(og + 1) * 512].rearrange("(k p) c -> p k c", p=128)
                    self.pc[("cout", j, og)] = self.add_piece(src, [128, 16, 512])
                    per_tile.append(self.pc[("cout", j, og)])
            else:
                per_tile += self.attn_pieces(j)
            for hh in range(2):
                for fg in range(hh * 8, hh * 8 + 8):
                    src = W["mlp_w1"][layer][:, fg * 512:(fg + 1) * 512].rearrange("(k p) c -> p k c", p=128)
                    self.pc[("w1", layer, fg)] = self.add_piece(src, [128, 16, 512])
                    per_tile.append(self.pc[("w1", layer, fg)])
                for half in range(2):
                    for kg in range(hh * 4, hh * 4 + 4):
                        src = W["mlp_w2"][layer][kg * 1024:(kg + 1) * 1024,
                                                 half * 1024:(half + 1) * 1024].rearrange("(k p) c -> p k c", p=128)
                        self.pc[("w2", layer, half, kg)] = self.add_piece(src, [128, 8, 1024])
                        per_tile.append(self.pc[("w2", layer, half, kg)])
            order += per_tile * self.ntile
            cast_marks.append((n0, len(self.P.lists["pool"]), dict(self.P.dma_count)))
        self.use_order = order
        for n0, n1, cnt in cast_marks:
            for op in self.P.lists["pool"][n0:n1]:
                if op.dsem is not None and op.dsem >= 100:
                    op.seq = self.P.dma_count[op.dsem]
        self.wpos = 0
        self.wissued = 0
        self.whold = None

        nl = len(self.layers)
        for li, layer in enumerate(self.layers):
            for tile in range(self.ntile):
                if li == 0:
                    self.load_x_tile(tile)
                else:
                    self.load_R(tile)
                if layer % 2 == 0:
                    self.conv_mixer(layer, tile % (SEQ // TT) == 0)
                else:
                    self.attn_mixer(layer, tile)
                if self.debug == "attn_only":
                    continue
                self.mlp(layer)
                if li == nl - 1:
                    self.final_out(tile)
                else:
                    self.store_R(tile)
        if nl == 0:
            for tile in range(self.ntile):
                self.load_x_tile(tile)
                self.final_out(tile)
        self.P.emit(st)
        st.close()
        return nc

    def attn_alloc(self):
        if not any(l % 2 == 1 for l in self.layers):
            return
        self.CKVT = self.sb(2 * SEQ * 2, BF16, [2, SEQ])
        self.CKVtok = self.sb(16 * 256 * 2, BF16, [16, 256])
        self.KI = self.sb(SEQ * 2, BF16)
        self.CQ = self.sb(4 * TT * 2, BF16, [4, TT])
        self.MASKT = self.sb(16 * TT * 2, BF16, [16, TT])
        self.thr_rep = self.sb(TT * 4, F32)
        self.vecs_a = self.sb(4 * 8 * 4, F32, [4, 8])
        self.wtok = self.sb(4 * 16 * 4, F32, [4, 16])
        self.wcol = self.sb(16 * 4, F32)
        self.rel = [self.sb(TT * 2, BF16) for _ in range(3)]
        base = (self.sb_off + CELL - 1) // CELL * CELL
        self.cq_raw = self.sb(4 * TT * 4, F32, [4, TT])
        self.kv_raw = self.sb(2 * TT * 4, F32, [2, TT])
        self.ki_raw = self.sb(TT * 4, F32)
        o = base
        self.junk = self.sb(SEQ * 2, BF16, at=o); o += SEQ * 2
        self.Amat = self.sb(128 * 4, F32, [8, 16], at=o); o += 512
        self.dg = self.sb(128 * 4, F32, at=o); o += 512
        self.tmpdiag = self.sb(TT * 4, F32, at=o); o += TT * 4
        o = base
        self.QH = self.sb(TT * 2, BF16, at=o); o += TT * 2
        self.QL = self.sb(2 * TT * 2, BF16, [2, TT], at=o); o += 2 * TT * 2
        self.E = [self.sb(TT * 2, BF16, at=o + i * TT * 2) for i in range(2)]; o += 2 * TT * 2
        self.Pm = [self.sb(TT * 2, BF16, at=o + i * TT * 2) for i in range(2)]; o += 2 * TT * 2
        self.rden = self.sb(TT * 4, F32, at=o); o += TT * 4
        self.OLn = self.sb(2 * TT * 2, BF16, [2, TT], at=o); o += 2 * TT * 2
        assert o <= self.sb_off
        o = self.hbuf.lo
        self.SC = [self.sb(SEQ * 4, F32, at=o + i * SEQ * 4) for i in range(2)]; o += 2 * SEQ * 4
        self.Qi = [self.sb(128 * 16 * 2, BF16, [128, 16], at=o + i * 4096) for i in range(2)]; o += 8192
        self.Z = [self.sb(16 * 128 * 2, BF16, [16, 128], at=o + i * 4096) for i in range(2)]; o += 8192
        assert o <= self.hbuf.hi

    def attn_pieces(self, j):
        W = self.W
        lst = []

        def reg(key, src, shape):
            self.pc[key] = self.add_piece(src, shape)
            lst.append(self.pc[key])
        reg(("ainA", j), W["attn_in"][j][:, 0:512].rearrange("(k p) c -> p k c", p=128), [128, 16, 512])
        reg(("ainB", j), W["attn_in"][j][:, 512:848].rearrange("(k p) c -> p k c", p=128), [128, 16, 336])
        reg(("wqidx", j), W["w_qidx"][j].rearrange("(k p) c -> p k c", p=128), [128, 4, 1024])
        reg(("wuq", j), W["w_uq"][j].rearrange("(k p) c -> p k c", p=128), [128, 4, 2048])
        reg(("wuk", j), W["w_uk"][j].rearrange("h d c -> d h c"), [128, 16, 256])
        reg(("wuv", j), [W["w_uv"][j][:, cc * 128:(cc + 1) * 128, :].rearrange("h p v -> p h v") for cc in range(2)],
            [128, 2, 16, 128])
        for og in range(4):
            reg(("aout", j, og), W["attn_out"][j][:, og * 512:(og + 1) * 512].rearrange("(k p) c -> p k c", p=128),
                [128, 16, 512])
        return lst

    def rmsnorm_l(self, srcs, gname, inv_n, dsts):
        ps = self.next_bank()
        n = len(srcs)
        for c in range(n):
            sq = self.sqt[c % 2]
            self.act(sq, srcs[c], AF.Square)
            self.mm(ps, self.ones_bf, sq, c == 0, c == n - 1)
        self.act(self.rstd, ps, AF.Ln, scale=inv_n, bias=self.eps_t.ap, extra_reads=[self.eps_t])
        self.act(self.rstd, self.rstd, AF.Exp, scale=-0.5)
        for c in range(n):
            self.stt(dsts[c], srcs[c], self.vcol(gname, c), self.rstd, ALU.mult, ALU.mult, reads=[self.vecs])

    def attn_mixer(self, layer, tile):
        j = layer // 2
        qt = tile % (SEQ // TT)
        t0 = qt * TT
        nkc = 4 * (qt + 1)
        nk = nkc * 128
        R, xn = self.R, self.xn
        cc_ = self.ccols
        ones_f = self.cst.cols(cc_["ones"], cc_["ones"] + 128)
        NIT = 20
        self.rmsnorm(R, 16, ("norm_mix", layer), 1.0 / D, xn)
        slotA = self.wget(self.pc[("ainA", j)])
        for m in range(4):
            ps = self.next_bank()
            for kc in range(16):
                self.mm(ps, slotA.idx(kc).cols(m * 128, (m + 1) * 128), xn.idx(kc), kc == 0, kc == 15)
            self.copy("act" if m % 2 else "dve", self.cq_raw.idx(m), ps)
        slotB = self.wget(self.pc[("ainB", j)])
        for m in range(2):
            ps = self.next_bank()
            for kc in range(16):
                self.mm(ps, slotB.idx(kc).cols(m * 128, (m + 1) * 128), xn.idx(kc), kc == 0, kc == 15)
            self.copy("act" if m % 2 else "dve", self.kv_raw.idx(m), ps)
        ps = self.next_bank()
        for kc in range(16):
            self.mm(ps.parts(0, 64), slotB.idx(kc).cols(256, 320), xn.idx(kc), kc == 0, kc == 15)
        kir = self.ki_raw.parts(0, 64)
        self.copy("act", kir, ps.parts(0, 64))
        ps = self.next_bank()
        for tc in range(4):
            for kc in range(16):
                self.mm(ps.cols(tc * 16, (tc + 1) * 16), xn.idx(kc).cols(tc * 128, (tc + 1) * 128),
                        slotB.idx(kc).cols(320, 336), kc == 0, kc == 15)
        self.act(self.wtok.view(self.wtok.ap.rearrange("p a b -> p (a b)")), ps.cols(0, 64), AF.Identity,
                 scale=1.0 / 32.0)
        self.rmsnorm_l([self.cq_raw.idx(c) for c in range(4)], ("q_norm", j), 1.0 / 512, [self.CQ.idx(c) for c in range(4)])
        kvd = [self.CKVT.idx(c).cols(t0, t0 + TT) for c in range(2)]
        self.rmsnorm_l([self.kv_raw.idx(c) for c in range(2)], ("kv_norm", j), 1.0 / 256, kvd)
        mu = self.cq_raw.idx(0).parts(0, 64)
        var = self.cq_raw.idx(1).parts(0, 64)
        xc = self.cq_raw.idx(2).parts(0, 64)
        sqk = self.cq_raw.idx(3).parts(0, 64)
        on64 = ones_f.parts(0, 64).cols(0, 64)
        self.act(sqk, kir, AF.Square)
        pm = self.next_bank().parts(0, 64)
        self.mm(pm, on64, kir, True, True)
        pv = self.next_bank().parts(0, 64)
        self.mm(pv, on64, sqk, True, True)
        self.act(mu, pm, AF.Identity, scale=1.0 / 64)
        self.tt("dve", var, mu, mu, ALU.mult)
        self.stt(var, pv, 1.0 / 64, var, ALU.mult, ALU.subtract)
        self.act(var, var, AF.Ln, bias=self.eps_t.ap[0:64], extra_reads=[self.eps_t])
        self.act(var, var, AF.Exp, scale=-0.5)
        self.tt("dve", xc, kir, mu, ALU.subtract)
        self.tt("dve", xc, xc, var, ALU.mult)
        kid = self.KI.cols(t0, t0 + TT).parts(0, 64)
        self.ts("dve", kid, xc, self.vcol(("ln_g", j), 0, 1, 64), self.vcol(("ln_b", j), 0, 1, 64), ALU.mult, ALU.add,
                reads=[self.vecs])
        pb = self.next_bank(BF16)
        for i in range(4):
            for c in range(2):
                self.tr(pb.cols(i * 256 + c * 128, i * 256 + (c + 1) * 128),
                        self.CKVT.idx(c).cols(t0 + i * 128, t0 + (i + 1) * 128), self.ident_bf)
        dstk = self.CKVtok.view(self.CKVtok.ap[:, 4 * qt:4 * qt + 4, :].rearrange("p a b -> p (a b)"))
        dstk.lo = self.CKVtok.lo + 4 * qt * 512
        dstk.hi = dstk.lo + 4 * 512
        self.copy("dve", dstk, pb)
        slotQ = self.wget(self.pc[("wqidx", j)])
        for st in range(4):
            Qi, Z, SC = self.Qi[st % 2], self.Z[st % 2], self.SC[st % 2]
            va = self.vecs_a.idx(st)
            v_hi, v_lo, v_w0, v_mid, v_cnt, v_gew, v_t = [va.cols(i, i + 1) for i in range(7)]
            for hg in range(4):
                ps = self.bank(6 + hg % 2)
                for hh in range(4):
                    h = hg * 4 + hh
                    for kc in range(4):
                        self.mm(ps.parts(0, 64).cols(hh * 128, (hh + 1) * 128), slotQ.idx(kc).cols(h * 64, (h + 1) * 64),
                                self.CQ.idx(kc).cols(st * 128, (st + 1) * 128), kc == 0, kc == 3)
                dq = Qi.view(Qi.ap[0:64, :, hg * 4:hg * 4 + 4].rearrange("p t h -> p h t"))
                sq_ = ps.view(ps.ap[0:64, :].rearrange("p (h t) -> p h t", t=128))
                self.copy("act" if hg % 2 else "dve", dq, sq_)
            wt = self.wtok.idx(st)
            for tp in range(8):
                self.ts("dve", self.Amat.idx(tp), wt, self.cst.ap[:, cc_["m8"] + tp:cc_["m8"] + tp + 1], None, ALU.mult,
                        reads=[self.cst])
            pw = self.bank(6).cols(0, 16)
            self.mm(pw, self.Amat.view(self.Amat.ap.rearrange("p a b -> p (a b)")),
                    self.cst.cols(cc_["bsel"], cc_["bsel"] + 16), True, True)
            self.copy("dve", self.wcol, pw)
            self.P.add("pool", lambda e, Z=Z: e.memset(Z.ap, 0.0), writes=[Z])
            d8 = self.cst.cols(cc_["d8"], cc_["d8"] + 8)
            for g in range(16):
                zg = Z.idx(g).cols(8 * g, 8 * g + 8)
                self.ts("dve", zg, d8, self.wcol.ap[:, g:g + 1], None, ALU.mult, reads=[self.wcol])
            for sb_ in range(qt + 1):
                scb = self.bank(4 + (st * 4 + sb_) % 2)
                kis = self.KI.cols(sb_ * TT, (sb_ + 1) * TT).parts(0, 64)

                def s1(g):
                    lq = Qi.view(Qi.ap[0:64, 8 * g:8 * g + 8, :].rearrange("p t h -> p (t h)"))
                    self.mm(self.bank(g % 4), lq, kis, True, True)
                s1(0)
                for g in range(16):
                    if g + 1 < 16:
                        s1(g + 1)
                    rel = self.rel[g % 3]
                    self.act(rel, self.bank(g % 4), AF.Relu)
                    self.mm(scb, Z.idx(g), rel, g == 0, g == 15)
                scd = SC.cols(sb_ * TT, (sb_ + 1) * TT)
                if sb_ == qt:
                    self.tt("dve", scd, scb, self.cst.cols(cc_[("cb", st)], cc_[("cb", st)] + TT), ALU.add)
                else:
                    self.copy("act", scd, scb)
            if qt == 0 and st < 2:
                self.P.add("dve", lambda e, v=v_lo: e.memset(v.ap, -1.0e29), writes=[v_lo])
            else:
                scv = SC.cols(0, nk)
                self.P.add("dve", lambda e, o=v_hi, i=scv: e.tensor_reduce(o.ap, i.ap, AX.X, ALU.max),
                           reads=[scv], writes=[v_hi])
                cbt = self.cst.cols(cc_[("cb", st)], cc_[("cb", st)] + TT)
                self.stt(self.tmpdiag, cbt, -2.0, SC.cols(qt * TT, nk), ALU.mult, ALU.add)
                self.P.add("dve", lambda e, o=v_lo, i=self.tmpdiag: e.tensor_reduce(o.ap, i.ap, AX.X, ALU.min),
                           reads=[self.tmpdiag], writes=[v_lo])
                if qt > 0:
                    scf = SC.cols(0, qt * TT)
                    self.P.add("dve", lambda e, o=v_t, i=scf: e.tensor_reduce(o.ap, i.ap, AX.X, ALU.min),
                               reads=[scf], writes=[v_t])
                    self.tt("dve", v_lo, v_lo, v_t, ALU.min)
                self.tt("dve", v_w0, v_hi, v_lo, ALU.subtract)
                jk = self.junk.cols(0, nk)
                for k in range(NIT):
                    f = 2.0 ** -(k + 1)
                    self.stt(v_mid, v_w0, f, v_lo, ALU.mult, ALU.add)
                    self.P.add("dve", lambda e, o=jk, i=scv, m=v_mid, c=v_cnt: e.tensor_scalar(
                        o.ap, i.ap, m.ap, None, ALU.is_ge, op1=ALU.add, accum_out=c.ap),
                        reads=[scv, v_mid, jk], writes=[v_cnt])
                    self.ts("dve", v_gew, v_cnt, 255.5, f, ALU.is_ge, ALU.mult)
                    self.stt(v_lo, v_gew, v_w0.ap, v_lo, ALU.mult, ALU.add, reads=[v_w0])
            self.ts("dve", self.dg, self.ident_f, v_lo.ap, None, ALU.mult, reads=[v_lo])
            pt = self.bank(7)
            for r in range(4):
                self.mm(pt.cols(r * 128, (r + 1) * 128), ones_f, self.dg, True, True)
            self.copy("act", self.thr_rep, pt)
            for scg in range(qt + 1):
                pT = self.bank(6 + scg % 2)
                for i in range(4):
                    sc = scg * 4 + i
                    self.tr(pT.cols(i * 128, (i + 1) * 128), SC.cols(sc * 128, (sc + 1) * 128), self.ident_f)
                mo = self.MASKT.view(self.MASKT.ap[:, scg * 4:scg * 4 + 4, st * 128:(st + 1) * 128])
                mo.lo = self.MASKT.lo + scg * 4 * TT * 2
                mo.hi = mo.lo + 4 * TT * 2
                pin = pT.view(pT.ap.rearrange("p (a b) -> p a b", b=128))
                tin = self.thr_rep.view(self.thr_rep.ap.rearrange("p (a b) -> p a b", b=128))
                self.tt("dve", mo, pin, tin, ALU.is_ge)
        sUQ = self.wget(self.pc[("wuq", j)], hold=True)
        sUK = self.wget(self.pc[("wuk", j)])
        sUV = self.wget(self.pc[("wuv", j)])
        O = xn
        scale = 128.0 ** -0.5
        for h in range(16):
            b6 = self.bank(6)
            for kc in range(4):
                self.mm(b6, sUQ.idx(kc).cols(h * 128, (h + 1) * 128), self.CQ.idx(kc), kc == 0, kc == 3)
            self.copy("act", self.QH, b6)
            for c in range(2):
                bq = self.bank(7 - c)
                self.mm(bq, sUK.idx(h).cols(c * 128, (c + 1) * 128), self.QH, True, True)
                self.act(self.QL.idx(c), bq, AF.Identity, scale=scale)

            def qk(sc):
                bs = self.bank(3 + sc % 3)
                for c in range(2):
                    self.mm(bs, self.CKVT.idx(c).cols(sc * 128, (sc + 1) * 128), self.QL.idx(c), c == 0, c == 1)
            qk(0)
            for sc in range(nkc):
                if sc + 1 < nkc:
                    qk(sc + 1)
                E, Pm = self.E[sc % 2], self.Pm[sc % 2]
                self.act(E, self.bank(3 + sc % 3), AF.Exp)
                self.tt("pool" if sc % 2 else "dve", Pm, E, self.MASKT.idx(sc), ALU.mult)
                for c in range(2):
                    self.mm(self.bank(c), self.CKVtok.idx(sc).cols(c * 128, (c + 1) * 128), Pm, sc == 0, sc == nkc - 1)
                self.mm(self.bank(2), self.ones_bf, Pm, sc == 0, sc == nkc - 1)
            self.P.add("dve", lambda e, o=self.rden, i=self.bank(2): e.reciprocal(o.ap, i.ap),
                       reads=[self.bank(2)], writes=[self.rden])
            for c in range(2):
                self.tt("dve", self.OLn.idx(c), self.bank(c), self.rden, ALU.mult)
            bo = self.bank(6)
            for c in range(2):
                self.mm(bo, sUV.view(sUV.ap[:, c, h, :]), self.OLn.idx(c), c == 0, c == 1)
            self.copy("act", O.idx(h), bo)
        self.ps_rr = 3
        self.whold = None
        for og in range(4):
            slot = self.wget(self.pc[("aout", j, og)])
            for dd in range(4):
                d = og * 4 + dd
                ps = self.next_bank()
                for kc in range(16):
                    self.mm(ps, slot.idx(kc).cols(dd * 128, (dd + 1) * 128), O.idx(kc), kc == 0, kc == 15)
                self.tt("dve", R.idx(d), ps, R.idx(d), ALU.add)


_CACHE = {}


def get_prog(nseq, layers):
    key = (nseq, tuple(layers))
    if key not in _CACHE:
        b = Builder(nseq=nseq, layers=layers)
        _CACHE[key] = b.build()
    return _CACHE[key]


def make_inputs(inputs, nseq, ncores):
    x = np.ascontiguousarray(np.asarray(inputs["x"], np.float32))
    shared = {"vecs": build_vecs(inputs), "consts": build_consts()}
    for name in ("mlp_w1", "mlp_w2", "conv_in", "conv_out", "attn_in", "attn_out"):
        shared[name] = np.ascontiguousarray(np.asarray(inputs[name], np.float32))
    shared["w_uq"] = np.ascontiguousarray(np.asarray(inputs["w_uq"], np.float32)).reshape(2, 512, 2048)
    shared["w_qidx"] = np.ascontiguousarray(np.asarray(inputs["w_qidx"], np.float32)).reshape(2, 512, 1024)
    shared["w_uk"] = np.ascontiguousarray(np.asarray(inputs["w_uk"], np.float32))
    shared["w_uv"] = np.ascontiguousarray(np.asarray(inputs["w_uv"], np.float32))
    in_maps = []
    for c in range(ncores):
        m = dict(shared)
        m["x"] = x[c * nseq:(c + 1) * nseq].reshape(nseq * SEQ, D)
        in_maps.append(m)
    return in_maps


def run(inputs, nseq=2, layers=(0, 1, 2, 3), ncores=NCORES):
    nc = get_prog(nseq, layers)
    in_maps = make_inputs(inputs, nseq, ncores)
    if not layers:
        in_maps = [{k: v for k, v in m.items() if k in ("x", "vecs", "consts")} for m in in_maps]
    elif not any(l % 2 == 1 for l in layers):
        in_maps = [{k: v for k, v in m.items() if not (k.startswith("w_") or k.startswith("attn"))} for m in in_maps]
    res = run_bass_kernel_spmd(nc, in_maps, core_ids=list(range(ncores)))
    out = np.stack([np.asarray(r["y"]).reshape(nseq, SEQ, D) for r in res.results], 0)
    return out.reshape(ncores * nseq, SEQ, D).astype(np.float32)


def kernel(**inputs):
    return run(inputs)
```

```python
from contextlib import ExitStack
import numpy as np
import concourse.bass as bass
import concourse.mybir as mybir
from concourse.bass_utils import run_bass_kernel_spmd

F32 = mybir.dt.float32
BF16 = mybir.dt.bfloat16
U8 = mybir.dt.uint8
ALU = mybir.AluOpType
AF = mybir.ActivationFunctionType
AX = mybir.AxisListType

D = 2048
DFF = 8192
SEQ = 2048
NCORES = 8
TT = 512
EPS = 1e-6
NEG = -1.0e30
SLOT_BYTES = 16384
NSLOT = 3
CELL = 256
SAME_ENG_SYNC = True
ESZ = {F32: 4, BF16: 2, U8: 1}


class T:
    __slots__ = ("ap", "space", "lo", "hi", "esz")

    def __init__(self, ap, space, lo, hi, esz):
        self.ap, self.space, self.lo, self.hi, self.esz = ap, space, lo, hi, esz

    def idx(self, i):
        n = self.ap.shape[1]
        sz = (self.hi - self.lo) // n
        return T(self.ap[:, i], self.space, self.lo + i * sz, self.lo + (i + 1) * sz, self.esz)

    def cols(self, a, b):
        assert len(self.ap.shape) == 2
        return T(self.ap[:, a:b], self.space, self.lo + a * self.esz, self.lo + b * self.esz, self.esz)

    def parts(self, a, b):
        return T(self.ap[a:b], self.space, self.lo, self.hi, self.esz)

    def view(self, ap):
        return T(ap, self.space, self.lo, self.hi, self.esz)


class DR:
    __slots__ = ("ap", "key")

    def __init__(self, ap, key):
        self.ap, self.key = ap, key


class Op:
    __slots__ = ("eng", "fn", "deps", "key", "seq", "sig", "val", "dsem")


class Prog:
    ENGS = ("pe", "act", "dve", "pool", "sp")

    def __init__(self, nc):
        self.nc = nc
        self.lists = {e: [] for e in self.ENGS}
        self.cells = {}
        self.nseq = {e: 0 for e in self.ENGS}
        self.dma_count = {}
        self.out_dma = {}

    def _cells(self, t):
        if isinstance(t, DR):
            return [("dram", t.key)]
        if t.space == "ps":
            return [("ps", c) for c in range(t.lo // 2048, (t.hi + 2047) // 2048)]
        return [("sb", c) for c in range(t.lo // CELL, (t.hi + CELL - 1) // CELL)]

    def add(self, eng, fn, reads=(), writes=(), dsem=None, is_out=False):
        op = Op()
        op.eng, op.fn, op.sig, op.val, op.dsem = eng, fn, False, 0, dsem
        if dsem is not None:
            op.key = ("dma", dsem)
            self.dma_count[dsem] = self.dma_count.get(dsem, 0) + 16
            op.seq = self.dma_count[dsem]
            if is_out:
                self.out_dma[dsem] = op.seq
        else:
            op.key = eng
            self.nseq[eng] += 1
            op.seq = self.nseq[eng]
        deps = {}

        def need(o):
            if o is None or o is op:
                return
            if o.dsem is None and op.dsem is None and o.eng == eng:
                if eng == "pe" or not SAME_ENG_SYNC:
                    return
            cur = deps.get(o.key)
            if cur is None or cur.seq < o.seq:
                deps[o.key] = o

        for t in reads:
            for c in self._cells(t):
                st = self.cells.get(c)
                if st is None:
                    st = self.cells[c] = [None, {}]
                need(st[0])
                cur = st[1].get(op.key)
                if cur is None or cur.seq < op.seq:
                    st[1][op.key] = op
        for t in writes:
            for c in self._cells(t):
                st = self.cells.get(c)
                if st is None:
                    st = self.cells[c] = [None, {}]
                need(st[0])
                for r in st[1].values():
                    need(r)
                st[0] = op
                st[1] = {}
        op.deps = list(deps.values())
        for o in op.deps:
            if o.dsem is None:
                o.sig = True
        self.lists[eng].append(op)
        return op

    def emit(self, stack):
        nc = self.nc
        sems = {e: stack.enter_context(nc.semaphore("sem_" + e)) for e in self.ENGS}
        dsems = {k: stack.enter_context(nc.semaphore("dsem_%s" % str(k))) for k in self.dma_count}
        for e in self.ENGS:
            cnt = 0
            for op in self.lists[e]:
                if op.dsem is None and op.sig:
                    cnt += 1
                    op.val = cnt
        block = stack.enter_context(nc.Block())

        def run(ename, eh):
            known = {}
            for op in self.lists[ename]:
                for o in op.deps:
                    if o.dsem is None:
                        s, v = sems[o.eng], o.val
                    else:
                        s, v = dsems[o.dsem], o.seq
                    if known.get(id(s), 0) >= v:
                        continue
                    known[id(s)] = v
                    eh.wait_ge(s, v)
                ins = op.fn(eh)
                if op.dsem is not None:
                    ins.then_inc(dsems[op.dsem], 16)
                elif op.sig:
                    ins.then_inc(sems[ename], 1)
            if ename == "sp":
                for k, v in self.out_dma.items():
                    eh.wait_ge(dsems[k], v)

        @block.tensor
        def _(e):
            run("pe", e)

        @block.scalar
        def _(e):
            run("act", e)

        @block.vector
        def _(e):
            run("dve", e)

        @block.gpsimd
        def _(e):
            run("pool", e)

        @block.sync
        def _(e):
            run("sp", e)


def vec_layout():
    cols = {}
    n = 0

    def put(name, k):
        nonlocal n
        cols[name] = n
        n += k
    for i in range(4):
        put(("norm_mix", i), 16)
        put(("norm_mlp", i), 16)
    put(("final_norm",), 16)
    for j in range(2):
        for k in range(3):
            put(("conv_w", j, k), 16)
        put(("q_norm", j), 4)
        put(("kv_norm", j), 2)
        put(("ln_g", j), 1)
        put(("ln_b", j), 1)
    return cols, n


def const_layout():
    cols = {}
    n = 0

    def put(name, k):
        nonlocal n
        cols[name] = n
        n += k
    put("ident", 128)
    put("ones", 128)
    for s in range(4):
        put(("cb", s), 512)
    put("m8", 8)
    put("d8", 8)
    put("bsel", 16)
    return cols, n


def build_vecs(inp):
    cols, n = vec_layout()
    v = np.zeros((128, n), np.float32)

    def setv(name, arr):
        arr = np.asarray(arr, np.float32).reshape(-1)
        k = arr.size // 128
        if k == 0:
            v[:arr.size, cols[name]] = arr
        else:
            v[:, cols[name]:cols[name] + k] = arr.reshape(k, 128).T
    for i in range(4):
        setv(("norm_mix", i), inp["norm_mix"][i])
        setv(("norm_mlp", i), inp["norm_mlp"][i])
    setv(("final_norm",), inp["final_norm"])
    for j in range(2):
        for k in range(3):
            setv(("conv_w", j, k), inp["conv_w"][j, k])
        setv(("q_norm", j), inp["q_norm"][j])
        setv(("kv_norm", j), inp["kv_norm"][j])
        setv(("ln_g", j), inp["kidx_ln_g"][j])
        setv(("ln_b", j), inp["kidx_ln_b"][j])
    return v


def build_consts():
    cols, n = const_layout()
    c = np.zeros((128, n), np.float32)
    p = np.arange(128)
    c[:, cols["ident"]:cols["ident"] + 128] = np.eye(128, dtype=np.float32)
    c[:, cols["ones"]:cols["ones"] + 128] = 1.0
    s = np.arange(512)
    for st in range(4):
        vis = s[None, :] <= (st * 128 + p)[:, None]
        c[:, cols[("cb", st)]:cols[("cb", st)] + 512] = np.where(vis, 0.0, NEG)
    c[:, cols["m8"]:cols["m8"] + 8] = (p[:, None] % 8 == np.arange(8)[None, :])
    c[:, cols["d8"]:cols["d8"] + 8] = (p[:, None] // 16 == np.arange(8)[None, :])
    c[:, cols["bsel"]:cols["bsel"] + 16] = (p[:, None] // 8 == np.arange(16)[None, :])
    return c


class Builder:
    def __init__(self, nseq=2, layers=(0, 1, 2, 3), do_final=True, debug=None, ntile=None):
        self.nseq = nseq
        self.ntok = nseq * SEQ
        self.ntile = ntile or self.ntok // TT
        self.layers = tuple(layers)
        self.do_final = do_final
        self.debug = debug
        self.nc = bass.Bass("TRN2", target_bir_lowering=False)
        self.P = Prog(self.nc)
        self.vcols, self.nv = vec_layout()
        self.ccols, self.ncc = const_layout()
        self.sb_off = 0
        self.ps_rr = 0
        self.pieces = []
        self.use_order = []
        self.dsem_n = 0

    def new_dsem(self):
        self.dsem_n += 1
        return self.dsem_n

    def sb(self, nbytes, dtype, shape=None, at=None):
        esz = ESZ[dtype]
        if at is None:
            lo = (self.sb_off + CELL - 1) // CELL * CELL
            self.sb_off = lo + nbytes
            assert self.sb_off <= self.arena_bytes, ("SBUF overflow", self.sb_off)
        else:
            lo = at
        ap = self.arena[:, lo:lo + nbytes].bitcast(dtype)
        if shape is not None and len(shape) == 2:
            ap = ap.rearrange("p (a b) -> p a b", b=shape[1])
        elif shape is not None and len(shape) == 3:
            ap = ap.rearrange("p (a b c) -> p a b c", b=shape[1], c=shape[2])
        return T(ap, "sb", lo, lo + nbytes, esz)

    def bank(self, i, dtype=F32):
        ap = self.psum[:, i * 512:(i + 1) * 512]
        if dtype != F32:
            ap = ap.bitcast(dtype)
        return T(ap, "ps", i * 2048, (i + 1) * 2048, ESZ[dtype])

    def next_bank(self, dtype=F32):
        b = self.bank(self.ps_rr % 8, dtype)
        self.ps_rr += 1
        return b

    def mm(self, out, lhsT, rhs, start, stop):
        self.P.add("pe", lambda e: e.matmul(out.ap, lhsT.ap, rhs.ap, start=start, stop=stop),
                   reads=[lhsT, rhs], writes=[out])

    def tr(self, out, in_, ident):
        self.P.add("pe", lambda e: e.transpose(out.ap, in_.ap, ident.ap), reads=[in_, ident], writes=[out])

    def act(self, out, in_, func, scale=1.0, bias=0.0, extra_reads=()):
        self.P.add("act", lambda e: e.activation(out.ap, in_.ap, func, bias=bias, scale=scale),
                   reads=[in_] + list(extra_reads), writes=[out])

    def tt(self, eng, out, a, b, op):
        h = "dve" if eng == "dve" else "pool"
        self.P.add(h, lambda e: e.tensor_tensor(out.ap, a.ap, b.ap, op), reads=[a, b], writes=[out])

    def ts(self, eng, out, a, s1, s2, op0, op1=None, reads=(), accum=None):
        kw = {}
        if op1 is not None:
            kw["op1"] = op1
        w = [out]
        if accum is not None:
            kw["accum_out"] = accum.ap
            w.append(accum)
        self.P.add(eng, lambda e: e.tensor_scalar(out.ap, a.ap, s1, s2, op0, **kw),
                   reads=[a] + list(reads), writes=w)

    def stt(self, out, a, scalar, b, op0, op1, reads=()):
        self.P.add("dve", lambda e: e.scalar_tensor_tensor(out.ap, a.ap, scalar, b.ap, op0, op1),
                   reads=[a, b] + list(reads), writes=[out])

    def copy(self, eng, out, in_):
        if eng == "act":
            self.P.add("act", lambda e: e.copy(out.ap, in_.ap), reads=[in_], writes=[out])
        else:
            self.P.add(eng, lambda e: e.tensor_copy(out.ap, in_.ap), reads=[in_], writes=[out])

    def dma(self, q, out, in_, dsem, reads, writes, is_out=False):
        self.P.add(q, lambda e: e.dma_start(out=out, in_=in_), reads=reads, writes=writes, dsem=dsem,
                   is_out=is_out)

    def vcol(self, name, c=0, n=1, parts=128):
        o = self.vcols[name] + c
        return self.vecs.ap[0:parts, o:o + n]

    def add_piece(self, src_ap, shape):
        n = int(np.prod(shape[1:]))
        assert n * 2 <= SLOT_BYTES
        idx = len(self.pieces)
        scr = self.nc.dram_tensor("wscr%d" % idx, [128, n], BF16, kind="Internal").ap()
        res = DR(scr, ("w", idx))
        sem = 100 + 2 * self.cast_group + (idx % 2)
        if len(shape) == 3:
            self.dma("pool", scr.rearrange("p (a b) -> p a b", b=shape[2]), src_ap, sem, reads=[], writes=[res])
        else:
            sub = shape[2] * shape[3]
            for g in range(shape[1]):
                dst = scr[:, g * sub:(g + 1) * sub].rearrange("p (a b) -> p a b", b=shape[3])
                self.dma("pool", dst, src_ap[g], sem, reads=[], writes=[DR(None, ("w", idx, g))])
        self.pieces.append((res, n, shape))
        return idx

    def wget(self, idx, hold=False):
        pos = self.wpos
        assert self.use_order[pos] == idx, (pos, idx, self.use_order[pos])
        if hold and self.whold is None:
            self.whold = pos
        base = pos if self.whold is None else self.whold
        while self.wissued < min(len(self.use_order), base + NSLOT):
            k = self.wissued
            res, n, shape = self.pieces[self.use_order[k]]
            s = k % NSLOT
            st = self.slots[s]
            dst = st.ap[:, 0:n]
            rr = [res] if len(shape) == 3 else [DR(None, ("w", self.use_order[k], g)) for g in range(shape[1])]
            self.dma("sp", dst, res.ap, 200 + s, reads=rr, writes=[T(dst, "sb", st.lo, st.lo + 2 * n, 2)])
            self.wissued += 1
        self.wpos += 1
        res, n, shape = self.pieces[idx]
        st = self.slots[pos % NSLOT]
        ap = st.ap[:, 0:n]
        if len(shape) == 3:
            ap = ap.rearrange("p (a b) -> p a b", b=shape[2])
        elif len(shape) == 4:
            ap = ap.rearrange("p (a b c) -> p a b c", b=shape[2], c=shape[3])
        return T(ap, "sb", st.lo, st.lo + 2 * n, 2)

    def rmsnorm(self, src, nch, gname, inv_n, dst, ones_parts=128, width=TT):
        ps = self.next_bank()
        for c in range(nch):
            sq = self.sqt[c % 2]
            self.act(sq, src.idx(c), AF.Square)
            self.mm(ps, self.ones_bf, sq, c == 0, c == nch - 1)
        self.act(self.rstd, ps, AF.Ln, scale=inv_n, bias=self.eps_t.ap, extra_reads=[self.eps_t])
        self.act(self.rstd, self.rstd, AF.Exp, scale=-0.5)
        for c in range(nch):
            self.stt(dst.idx(c), src.idx(c), self.vcol(gname, c), self.rstd, ALU.mult, ALU.mult,
                     reads=[self.vecs])

    def mlp(self, layer):
        R, xn, h = self.R, self.xn, self.hbuf
        self.rmsnorm(R, 16, ("norm_mlp", layer), 1.0 / D, xn)
        for hh in range(2):
            for fg in range(hh * 8, hh * 8 + 8):
                slot = self.wget(self.pc[("w1", layer, fg)])
                for jj in range(4):
                    j = (fg - hh * 8) * 4 + jj
                    ps = self.next_bank()
                    for kc in range(16):
                        self.mm(ps, slot.idx(kc).cols(jj * 128, (jj + 1) * 128), xn.idx(kc), kc == 0, kc == 15)
                    tmp = self.rtmp[j % 2]
                    self.act(tmp, ps, AF.Relu)
                    self.tt("dve", h.idx(j), tmp, tmp, ALU.mult)
            for half in range(2):
                banks = [self.bank(i) for i in range(8)]
                for kg in range(hh * 4, hh * 4 + 4):
                    slot = self.wget(self.pc[("w2", layer, half, kg)])
                    for kk in range(8):
                        k = (kg - hh * 4) * 8 + kk
                        for dl in range(8):
                            self.mm(banks[dl], slot.idx(kk).cols(dl * 128, (dl + 1) * 128), h.idx(k), k == 0, k == 31)
                for dl in range(8):
                    d = half * 8 + dl
                    self.tt("dve", R.idx(d), banks[dl], R.idx(d), ALU.add)
        self.ps_rr = 0

    def conv_mixer(self, layer, first_in_seq):
        j = layer // 2
        R, xn = self.R, self.xn
        u = self.sb(16 * TT * 2, BF16, [16, TT], at=self.hbuf.lo)
        self.rmsnorm(R, 16, ("norm_mix", layer), 1.0 / D, xn)
        for dj in range(16):
            slot = self.wget(self.pc[("cin", j, dj)])
            bks = [self.next_bank() for _ in range(3)]
            for g in range(3):
                for kc in range(16):
                    lw = slot.view(slot.ap[:, g, kc, :])
                    self.mm(bks[g], lw, xn.idx(kc), kc == 0, kc == 15)
            z = self.zt[dj % 2]
            cgs = self.cgs[dj % 2]
            acc = self.acc[dj % 2]
            halo = self.halo.idx(dj)
            self.copy("act", cgs, bks[1])
            if first_in_seq:
                self.P.add("pool", lambda e, z=z: e.memset(z.ap[:, 0:2], 0.0), writes=[z.cols(0, 2)])
            else:
                self.copy("pool", z.cols(0, 2), halo)
            self.tt("dve", z.cols(2, TT + 2), bks[2], cgs, ALU.mult)
            self.ts("dve", acc, z.cols(2, TT + 2), self.vcol(("conv_w", j, 2), dj), None, ALU.mult,
                    reads=[self.vecs])
            self.stt(acc, z.cols(1, TT + 1), self.vcol(("conv_w", j, 1), dj), acc, ALU.mult, ALU.add,
                     reads=[self.vecs])
            self.stt(acc, z.cols(0, TT), self.vcol(("conv_w", j, 0), dj), acc, ALU.mult, ALU.add,
                     reads=[self.vecs])
            self.copy("pool", halo, z.cols(TT, TT + 2))
            self.tt("dve", u.idx(dj), bks[0], acc, ALU.mult)
        for og in range(4):
            slot = self.wget(self.pc[("cout", j, og)])
            for dd in range(4):
                d = og * 4 + dd
                ps = self.next_bank()
                for kc in range(16):
                    self.mm(ps, slot.idx(kc).cols(dd * 128, (dd + 1) * 128), u.idx(kc), kc == 0, kc == 15)
                self.tt("dve", R.idx(d), ps, R.idx(d), ALU.add)

    def load_x_tile(self, tile):
        xin = self.sb(4 * D * 4, F32, [4, D], at=self.hbuf.lo)
        src = self.x[tile * TT:(tile + 1) * TT, :].rearrange("(a p) d -> p a d", p=128)
        self.dma("sp", xin.ap, src, 1, reads=[], writes=[xin])
        for d in range(16):
            ps = self.next_bank()
            for tc in range(4):
                self.tr(ps.cols(tc * 128, (tc + 1) * 128), xin.idx(tc).cols(d * 128, (d + 1) * 128), self.ident_f)
            self.copy("act" if d % 2 else "dve", self.R.idx(d), ps)

    def final_out(self, tile):
        R = self.R
        xf = self.sb(16 * TT * 4, F32, [16, TT], at=self.hbuf.lo)
        self.rmsnorm(R, 16, ("final_norm",), 1.0 / D, xf)
        for tc in range(4):
            ot = self.otile[tc % 2]
            for dg in range(4):
                ps = self.next_bank()
                for dd in range(4):
                    self.tr(ps.cols(dd * 128, (dd + 1) * 128), xf.idx(dg * 4 + dd).cols(tc * 128, (tc + 1) * 128),
                            self.ident_f)
                self.copy("act" if dg % 2 else "dve", ot.cols(dg * 512, (dg + 1) * 512), ps)
            r0 = tile * TT + tc * 128
            self.dma("sp", self.y[r0:r0 + 128, :], ot.ap, 10 + tc % 2, reads=[ot], writes=[], is_out=True)

    def store_R(self, tile):
        dst = self.rs[:, :, tile * TT:(tile + 1) * TT].rearrange("c p t -> p c t")
        self.dma("sp", dst, self.R.ap, 2, reads=[self.R], writes=[DR(None, ("rs", tile))])

    def load_R(self, tile):
        src = self.rs[:, :, tile * TT:(tile + 1) * TT].rearrange("c p t -> p c t")
        self.dma("sp", self.R.ap, src, 3, reads=[DR(None, ("rs", tile))], writes=[self.R])

    def build(self):
        nc = self.nc
        st = ExitStack()
        self.stack = st
        ntok = self.ntok
        self.x = nc.dram_tensor("x", [ntok, D], F32, kind="ExternalInput").ap()
        self.y = nc.dram_tensor("y", [ntok, D], F32, kind="ExternalOutput").ap()
        vecs_d = nc.dram_tensor("vecs", [128, self.nv], F32, kind="ExternalInput").ap()
        consts_d = nc.dram_tensor("consts", [128, self.ncc], F32, kind="ExternalInput").ap()
        W = {}
        for name, shape in (("mlp_w1", [4, D, DFF]), ("mlp_w2", [4, DFF, D]), ("conv_in", [2, D, 3 * D]),
                            ("conv_out", [2, D, D]), ("attn_in", [2, D, 848]), ("w_uq", [2, 512, 2048]),
                            ("w_uk", [2, 16, 128, 256]), ("w_uv", [2, 16, 256, 128]),
                            ("w_qidx", [2, 512, 1024]), ("attn_out", [2, D, D])):
            if name.startswith("mlp") and not self.layers:
                continue
            if name.startswith("conv") and not any(l % 2 == 0 for l in self.layers):
                continue
            if (name.startswith("w_") or name.startswith("attn")) and not any(l % 2 == 1 for l in self.layers):
                continue
            W[name] = nc.dram_tensor(name, shape, F32, kind="ExternalInput").ap()
        self.W = W
        self.rs = nc.dram_tensor("rs", [16, 128, ntok], F32, kind="Internal").ap()

        self.arena_bytes = 212000
        arena_t = st.enter_context(nc.sbuf_tensor("arena", [128, self.arena_bytes], U8))
        self.arena = arena_t.ap() if hasattr(arena_t, "ap") else arena_t[:]
        psum_t = st.enter_context(nc.psum_tensor("psum", [128, 4096], F32))
        self.psum = psum_t.ap() if hasattr(psum_t, "ap") else psum_t[:]

        self.vecs = self.sb(self.nv * 4, F32)
        self.cst = self.sb(self.ncc * 4, F32)
        self.ident_f = self.cst.cols(self.ccols["ident"], self.ccols["ident"] + 128)
        self.ident_bf = self.sb(256, BF16)
        self.ones_bf = self.sb(256, BF16)
        self.eps_t = self.sb(4, F32)
        self.slots = [self.sb(SLOT_BYTES, BF16) for _ in range(NSLOT)]
        self.R = self.sb(16 * TT * 4, F32, [16, TT])
        self.xn = self.sb(16 * TT * 2, BF16, [16, TT])
        self.hbuf = self.sb(32 * TT * 2, BF16, [32, TT])
        self.sqt = [self.sb(TT * 2, BF16) for _ in range(2)]
        self.rtmp = [self.sb(TT * 2, BF16) for _ in range(2)]
        self.rstd = self.sb(TT * 4, F32)
        self.halo = self.sb(16 * 2 * 4, F32, [16, 2])
        o = self.hbuf.lo + 16 * TT * 2
        self.zt = [self.sb(2112, F32, at=o + i * 2112)for i in range(2)]
        self.zt = [T(z.ap[:, 0:TT + 2], "sb", z.lo, z.hi, 4) for z in self.zt]
        o += 2 * 2112
        self.cgs = [self.sb(TT * 4, F32, at=o + i * TT * 4) for i in range(2)]
        o += 2 * TT * 4
        self.acc = [self.sb(TT * 4, F32, at=o + i * TT * 4) for i in range(2)]
        self.otile = [self.sb(D * 4, F32, at=self.xn.lo + i * D * 4) for i in range(2)]
        self.attn_alloc()

        self.dma("sp", self.vecs.ap, vecs_d, 20, reads=[], writes=[self.vecs])
        self.dma("sp", self.cst.ap, consts_d, 21, reads=[], writes=[self.cst])
        self.copy("dve", self.ident_bf, self.ident_f)
        self.copy("dve", self.ones_bf, self.cst.cols(self.ccols["ones"], self.ccols["ones"] + 128))
        self.P.add("dve", lambda e: e.memset(self.eps_t.ap, EPS), writes=[self.eps_t])

        self.pc = {}
        order = []
        cast_marks = []
        for layer in self.layers:
            j = layer // 2
            per_tile = []
            n0 = len(self.P.lists["pool"])
            self.cast_group = len(cast_marks)
            if layer % 2 == 0:
                for dj in range(16):
                    src = [W["conv_in"][j][:, g * D + dj * 128:g * D + (dj + 1) * 128].rearrange(
                        "(k p) c -> p k c", p=128) for g in range(3)]
                    self.pc[("cin", j, dj)] = self.add_piece(src, [128, 3, 16, 128])
                    per_tile.append(self.pc[("cin", j, dj)])
                for og in range(4):
                    src = W["conv_out"][j][:, og * 512:(og + 1) * 512].rearrange("(k p) c -> p k c", p=128)
                    self.pc[("cout", j, og)] = self.add_piece(src, [128, 16, 512])
                    per_tile.append(self.pc[("cout", j, og)])
            else:
                per_tile += self.attn_pieces(j)
            for hh in range(2):
                for fg in range(hh * 8, hh * 8 + 8):
                    src = W["mlp_w1"][layer][:, fg * 512:(fg + 1) * 512].rearrange("(k p) c -> p k c", p=128)
                    self.pc[("w1", layer, fg)] = self.add_piece(src, [128, 16, 512])
                    per_tile.append(self.pc[("w1", layer, fg)])
                for half in range(2):
                    for kg in range(hh * 4, hh * 4 + 4):
                        src = W["mlp_w2"][layer][kg * 1024:(kg + 1) * 1024,
                                                 half * 1024:(half + 1) * 1024].rearrange("(k p) c -> p k c", p=128)
                        self.pc[("w2", layer, half, kg)] = self.add_piece(src, [128, 8, 1024])
                        per_tile.append(self.pc[("w2", layer, half, kg)])
            order += per_tile * self.ntile
            cast_marks.append((n0, len(self.P.lists["pool"]), dict(self.P.dma_count)))
        self.use_order = order
        for n0, n1, cnt in cast_marks:
            for op in self.P.lists["pool"][n0:n1]:
                if op.dsem is not None and op.dsem >= 100:
                    op.seq = self.P.dma_count[op.dsem]
        self.wpos = 0
        self.wissued = 0
        self.whold = None

        nl = len(self.layers)
        for li, layer in enumerate(self.layers):
            for tile in range(self.ntile):
                if li == 0:
                    self.load_x_tile(tile)
                else:
                    self.load_R(tile)
                if layer % 2 == 0:
                    self.conv_mixer(layer, tile % (SEQ // TT) == 0)
                else:
                    self.attn_mixer(layer, tile)
                if self.debug == "attn_only":
                    continue
                self.mlp(layer)
                if li == nl - 1:
                    self.final_out(tile)
                else:
                    self.store_R(tile)
        if nl == 0:
            for tile in range(self.ntile):
                self.load_x_tile(tile)
                self.final_out(tile)
        self.P.emit(st)
        st.close()
        return nc

    def attn_alloc(self):
        if not any(l % 2 == 1 for l in self.layers):
            return
        self.CKVT = self.sb(2 * SEQ * 2, BF16, [2, SEQ])
        self.CKVtok = self.sb(16 * 256 * 2, BF16, [16, 256])
        self.KI = self.sb(SEQ * 2, BF16)
        self.CQ = self.sb(4 * TT * 2, BF16, [4, TT])
        self.MASKT = self.sb(16 * TT * 2, BF16, [16, TT])
        self.thr_rep = self.sb(TT * 4, F32)
        self.vecs_a = [self.sb(8 * 4, F32) for _ in range(4)]
        self.wtok = self.sb(4 * 16 * 4, F32, [4, 16])
        self.wcol = self.sb(16 * 4, F32)
        self.rel = [self.sb(TT * 2, BF16) for _ in range(3)]
        base = (self.sb_off + CELL - 1) // CELL * CELL
        self.cq_raw = self.sb(4 * TT * 4, F32, [4, TT])
        self.kv_raw = self.sb(2 * TT * 4, F32, [2, TT])
        self.ki_raw = self.sb(TT * 4, F32)
        o = base
        self.junk = self.sb(SEQ * 2, BF16, at=o); o += SEQ * 2
        self.Amat = self.sb(128 * 4, F32, [8, 16], at=o); o += 512
        self.dg = self.sb(128 * 4, F32, at=o); o += 512
        self.tmpdiag = self.sb(TT * 4, F32, at=o); o += TT * 4
        o = base
        self.QH = self.sb(TT * 2, BF16, at=o); o += TT * 2
        self.QL = self.sb(2 * TT * 2, BF16, [2, TT], at=o); o += 2 * TT * 2
        self.E = [self.sb(TT * 2, BF16, at=o + i * TT * 2) for i in range(2)]; o += 2 * TT * 2
        self.Pm = [self.sb(TT * 2, BF16, at=o + i * TT * 2) for i in range(2)]; o += 2 * TT * 2
        self.rden = self.sb(TT * 4, F32, at=o); o += TT * 4
        self.OLn = self.sb(2 * TT * 2, BF16, [2, TT], at=o); o += 2 * TT * 2
        assert o <= self.sb_off
        o = self.hbuf.lo
        self.SC = [self.sb(SEQ * 4, F32, at=o + i * SEQ * 4) for i in range(2)]; o += 2 * SEQ * 4
        self.Qi = [self.sb(128 * 16 * 2, BF16, [128, 16], at=o + i * 4096) for i in range(2)]; o += 8192
        self.Z = [self.sb(16 * 128 * 2, BF16, [16, 128], at=o + i * 4096) for i in range(2)]; o += 8192
        assert o <= self.hbuf.hi

    def attn_pieces(self, j):
        W = self.W
        lst = []

        def reg(key, src, shape):
            self.pc[key] = self.add_piece(src, shape)
            lst.append(self.pc[key])
        reg(("ainA", j), W["attn_in"][j][:, 0:512].rearrange("(k p) c -> p k c", p=128), [128, 16, 512])
        reg(("ainB", j), W["attn_in"][j][:, 512:848].rearrange("(k p) c -> p k c", p=128), [128, 16, 336])
        reg(("wqidx", j), W["w_qidx"][j].rearrange("(k p) c -> p k c", p=128), [128, 4, 1024])
        reg(("wuq", j), W["w_uq"][j].rearrange("(k p) c -> p k c", p=128), [128, 4, 2048])
        reg(("wuk", j), W["w_uk"][j].rearrange("h d c -> d h c"), [128, 16, 256])
        reg(("wuv", j), [W["w_uv"][j][:, cc * 128:(cc + 1) * 128, :].rearrange("h p v -> p h v") for cc in range(2)],
            [128, 2, 16, 128])
        for og in range(4):
            reg(("aout", j, og), W["attn_out"][j][:, og * 512:(og + 1) * 512].rearrange("(k p) c -> p k c", p=128),
                [128, 16, 512])
        return lst

    def rmsnorm_l(self, srcs, gname, inv_n, dsts):
        ps = self.next_bank()
        n = len(srcs)
        for c in range(n):
            sq = self.sqt[c % 2]
            self.act(sq, srcs[c], AF.Square)
            self.mm(ps, self.ones_bf, sq, c == 0, c == n - 1)
        self.act(self.rstd, ps, AF.Ln, scale=inv_n, bias=self.eps_t.ap, extra_reads=[self.eps_t])
        self.act(self.rstd, self.rstd, AF.Exp, scale=-0.5)
        for c in range(n):
            self.stt(dsts[c], srcs[c], self.vcol(gname, c), self.rstd, ALU.mult, ALU.mult, reads=[self.vecs])

    def attn_mixer(self, layer, tile):
        j = layer // 2
        qt = tile % (SEQ // TT)
        t0 = qt * TT
        nkc = 4 * (qt + 1)
        nk = nkc * 128
        R, xn = self.R, self.xn
        cc_ = self.ccols
        ones_f = self.cst.cols(cc_["ones"], cc_["ones"] + 128)
        NIT = 20
        self.rmsnorm(R, 16, ("norm_mix", layer), 1.0 / D, xn)
        slotA = self.wget(self.pc[("ainA", j)])
        for m in range(4):
            ps = self.next_bank()
            for kc in range(16):
                self.mm(ps, slotA.idx(kc).cols(m * 128, (m + 1) * 128), xn.idx(kc), kc == 0, kc == 15)
            self.copy("act" if m % 2 else "dve", self.cq_raw.idx(m), ps)
        slotB = self.wget(self.pc[("ainB", j)])
        for m in range(2):
            ps = self.next_bank()
            for kc in range(16):
                self.mm(ps, slotB.idx(kc).cols(m * 128, (m + 1) * 128), xn.idx(kc), kc == 0, kc == 15)
            self.copy("act" if m % 2 else "dve", self.kv_raw.idx(m), ps)
        ps = self.next_bank()
        for kc in range(16):
            self.mm(ps.parts(0, 64), slotB.idx(kc).cols(256, 320), xn.idx(kc), kc == 0, kc == 15)
        kir = self.ki_raw.parts(0, 64)
        self.copy("act", kir, ps.parts(0, 64))
        ps = self.next_bank()
        for tc in range(4):
            for kc in range(16):
                self.mm(ps.cols(tc * 16, (tc + 1) * 16), xn.idx(kc).cols(tc * 128, (tc + 1) * 128),
                        slotB.idx(kc).cols(320, 336), kc == 0, kc == 15)
        self.act(self.wtok.view(self.wtok.ap.rearrange("p a b -> p (a b)")), ps.cols(0, 64), AF.Identity,
                 scale=1.0 / 32.0)
        self.rmsnorm_l([self.cq_raw.idx(c) for c in range(4)], ("q_norm", j), 1.0 / 512, [self.CQ.idx(c) for c in range(4)])
        kvd = [self.CKVT.idx(c).cols(t0, t0 + TT) for c in range(2)]
        self.rmsnorm_l([self.kv_raw.idx(c) for c in range(2)], ("kv_norm", j), 1.0 / 256, kvd)
        mu = self.cq_raw.idx(0).parts(0, 64)
        var = self.cq_raw.idx(1).parts(0, 64)
        xc = self.cq_raw.idx(2).parts(0, 64)
        sqk = self.cq_raw.idx(3).parts(0, 64)
        on64 = ones_f.parts(0, 64).cols(0, 64)
        self.act(sqk, kir, AF.Square)
        pm = self.next_bank().parts(0, 64)
        self.mm(pm, on64, kir, True, True)
        pv = self.next_bank().parts(0, 64)
        self.mm(pv, on64, sqk, True, True)
        self.act(mu, pm, AF.Identity, scale=1.0 / 64)
        self.tt("dve", var, mu, mu, ALU.mult)
        self.stt(var, pv, 1.0 / 64, var, ALU.mult, ALU.subtract)
        self.act(var, var, AF.Ln, bias=self.eps_t.ap[0:64], extra_reads=[self.eps_t])
        self.act(var, var, AF.Exp, scale=-0.5)
        self.tt("dve", xc, kir, mu, ALU.subtract)
        self.tt("dve", xc, xc, var, ALU.mult)
        kid = self.KI.cols(t0, t0 + TT).parts(0, 64)
        self.ts("dve", kid, xc, self.vcol(("ln_g", j), 0, 1, 64), self.vcol(("ln_b", j), 0, 1, 64), ALU.mult, ALU.add,
                reads=[self.vecs])
        pb = self.next_bank(BF16)
        for i in range(4):
            for c in range(2):
                self.tr(pb.cols(i * 256 + c * 128, i * 256 + (c + 1) * 128),
                        self.CKVT.idx(c).cols(t0 + i * 128, t0 + (i + 1) * 128), self.ident_bf)
        dstk = self.CKVtok.view(self.CKVtok.ap[:, 4 * qt:4 * qt + 4, :].rearrange("p a b -> p (a b)"))
        dstk.lo = self.CKVtok.lo + 4 * qt * 512
        dstk.hi = dstk.lo + 4 * 512
        self.copy("dve", dstk, pb)
        slotQ = self.wget(self.pc[("wqidx", j)])
        NITB = 13

        def S_steps(st):
            Qi, Z, SC = self.Qi[st % 2], self.Z[st % 2], self.SC[st % 2]
            steps = []

            def qi_group(hg):
                ps = self.bank(6 + hg % 2)
                for hh in range(4):
                    h = hg * 4 + hh
                    for kc in range(4):
                        self.mm(ps.parts(0, 64).cols(hh * 128, (hh + 1) * 128), slotQ.idx(kc).cols(h * 64, (h + 1) * 64),
                                self.CQ.idx(kc).cols(st * 128, (st + 1) * 128), kc == 0, kc == 3)
                dq = Qi.view(Qi.ap[0:64, :, hg * 4:hg * 4 + 4].rearrange("p t h -> p h t"))
                sq_ = ps.view(ps.ap[0:64, :].rearrange("p (h t) -> p h t", t=128))
                self.copy("act" if hg % 2 else "dve", dq, sq_)
            for hg in range(4):
                steps.append(lambda hg=hg: qi_group(hg))

            def wsel_a():
                wt = self.wtok.idx(st)
                for tp in range(8):
                    self.ts("dve", self.Amat.idx(tp), wt, self.cst.ap[:, cc_["m8"] + tp:cc_["m8"] + tp + 1], None,
                            ALU.mult, reads=[self.cst])
                pw = self.bank(6).cols(0, 16)
                self.mm(pw, self.Amat.view(self.Amat.ap.rearrange("p a b -> p (a b)")),
                        self.cst.cols(cc_["bsel"], cc_["bsel"] + 16), True, True)
                self.copy("dve", self.wcol, pw)
                self.P.add("pool", lambda e, Z=Z: e.memset(Z.ap, 0.0), writes=[Z])

            def wsel_b(g0):
                d8 = self.cst.cols(cc_["d8"], cc_["d8"] + 8)
                for g in range(g0, g0 + 8):
                    zg = Z.idx(g).cols(8 * g, 8 * g + 8)
                    self.ts("dve", zg, d8, self.wcol.ap[:, g:g + 1], None, ALU.mult, reads=[self.wcol])
            steps.append(wsel_a)
            steps.append(lambda: wsel_b(0))
            steps.append(lambda: wsel_b(8))
            for sb_ in range(qt + 1):
                scb = self.bank(4 + (st * 4 + sb_) % 2)
                kis = self.KI.cols(sb_ * TT, (sb_ + 1) * TT).parts(0, 64)

                def s1(g, kis=kis):
                    lq = Qi.view(Qi.ap[0:64, 8 * g:8 * g + 8, :].rearrange("p t h -> p (t h)"))
                    self.mm(self.bank(g % 4), lq, kis, True, True)

                def gstep(g, scb=scb, sb_=sb_, s1=s1):
                    if g == 0:
                        s1(0)
                        s1(1)
                    if g + 2 < 16:
                        s1(g + 2)
                    rel = self.rel[g % 3]
                    self.act(rel, self.bank(g % 4), AF.Relu)
                    self.mm(scb, Z.idx(g), rel, g == 0, g == 15)
                    if g == 15:
                        scd = SC.cols(sb_ * TT, (sb_ + 1) * TT)
                        if sb_ == qt:
                            self.tt("dve", scd, scb, self.cst.cols(cc_[("cb", st)], cc_[("cb", st)] + TT), ALU.add)
                        else:
                            self.copy("act", scd, scb)
                for g in range(16):
                    steps.append(lambda g=g, gstep=gstep: gstep(g))
            return steps

        def B_steps(st):
            SC = self.SC[st % 2]
            va = self.vecs_a[st]
            v_hi, v_lo, v_w0, v_mid, v_cnt, v_gew, v_t = [va.cols(i, i + 1) for i in range(7)]
            steps = []
            if qt == 0 and st < 2:
                steps.append(lambda: self.P.add("dve", lambda e, v=v_lo: e.memset(v.ap, -1.0e29), writes=[v_lo]))
                return steps
            scv = SC.cols(0, nk)

            def init():
                self.P.add("dve", lambda e, o=v_hi, i=scv: e.tensor_reduce(o.ap, i.ap, AX.X, ALU.max),
                           reads=[scv], writes=[v_hi])
                cbt = self.cst.cols(cc_[("cb", st)], cc_[("cb", st)] + TT)
                self.stt(self.tmpdiag, cbt, -2.0, SC.cols(qt * TT, nk), ALU.mult, ALU.add)
                self.P.add("dve", lambda e, o=v_lo, i=self.tmpdiag: e.tensor_reduce(o.ap, i.ap, AX.X, ALU.min),
                           reads=[self.tmpdiag], writes=[v_lo])
                if qt > 0:
                    scf = SC.cols(0, qt * TT)
                    self.P.add("dve", lambda e, o=v_t, i=scf: e.tensor_reduce(o.ap, i.ap, AX.X, ALU.min),
                               reads=[scf], writes=[v_t])
                    self.tt("dve", v_lo, v_lo, v_t, ALU.min)
                self.tt("dve", v_w0, v_hi, v_lo, ALU.subtract)
            steps.append(init)
            jk = self.junk.cols(0, nk)

            def it(k):
                f = 2.0 ** -(k + 1)
                self.stt(v_mid, v_w0, f, v_lo, ALU.mult, ALU.add)
                self.P.add("dve", lambda e, o=jk, i=scv, m=v_mid, c=v_cnt: e.tensor_scalar(
                    o.ap, i.ap, m.ap, None, ALU.is_ge, op1=ALU.add, accum_out=c.ap),
                    reads=[scv, v_mid, jk], writes=[v_cnt])
                self.ts("dve", v_gew, v_cnt, 255.5, f, ALU.is_ge, ALU.mult)
                self.stt(v_lo, v_gew, v_w0.ap, v_lo, ALU.mult, ALU.add, reads=[v_w0])
            for k in range(NITB):
                steps.append(lambda k=k: it(k))
            return steps

        def M_steps(st):
            SC = self.SC[st % 2]
            v_lo = self.vecs_a[st].cols(1, 2)
            self.ts("dve", self.dg, self.ident_f, v_lo.ap, None, ALU.mult, reads=[v_lo])
            pt = self.bank(7)
            for r in range(4):
                self.mm(pt.cols(r * 128, (r + 1) * 128), ones_f, self.dg, True, True)
            self.copy("act", self.thr_rep, pt)
            for scg in range(qt + 1):
                pT = self.bank(6 + scg % 2)
                for i in range(4):
                    sc = scg * 4 + i
                    self.tr(pT.cols(i * 128, (i + 1) * 128), SC.cols(sc * 128, (sc + 1) * 128), self.ident_f)
                mo = self.MASKT.view(self.MASKT.ap[:, scg * 4:scg * 4 + 4, st * 128:(st + 1) * 128])
                mo.lo = self.MASKT.lo + scg * 4 * TT * 2
                mo.hi = mo.lo + 4 * TT * 2
                pin = pT.view(pT.ap.rearrange("p (a b) -> p a b", b=128))
                tin = self.thr_rep.view(self.thr_rep.ap.rearrange("p (a b) -> p a b", b=128))
                self.tt("dve", mo, pin, tin, ALU.is_ge)

        def merge(a, b):
            na, nb = len(a), len(b)
            ia = ib = 0
            while ia < na or ib < nb:
                if ia < na:
                    a[ia]()
                    ia += 1
                while ib < nb and (ia >= na or ib * na < ia * nb):
                    b[ib]()
                    ib += 1

        merge(S_steps(0), [])
        for st in range(1, 4):
            merge(S_steps(st), B_steps(st - 1))
            M_steps(st - 1)
        merge(B_steps(3), [])
        M_steps(3)
        sUQ = self.wget(self.pc[("wuq", j)], hold=True)
        sUK = self.wget(self.pc[("wuk", j)])
        sUV = self.wget(self.pc[("wuv", j)])
        O = xn
        scale = 128.0 ** -0.5
        for h in range(16):
            b6 = self.bank(6)
            for kc in range(4):
                self.mm(b6, sUQ.idx(kc).cols(h * 128, (h + 1) * 128), self.CQ.idx(kc), kc == 0, kc == 3)
            self.copy("act", self.QH, b6)
            for c in range(2):
                bq = self.bank(7 - c)
                self.mm(bq, sUK.idx(h).cols(c * 128, (c + 1) * 128), self.QH, True, True)
                self.act(self.QL.idx(c), bq, AF.Identity, scale=scale)

            def qk(sc):
                bs = self.bank(3 + sc % 3)
                for c in range(2):
                    self.mm(bs, self.CKVT.idx(c).cols(sc * 128, (sc + 1) * 128), self.QL.idx(c), c == 0, c == 1)
            qk(0)
            if nkc > 1:
                qk(1)
            for sc in range(nkc):
                if sc + 2 < nkc:
                    qk(sc + 2)
                E, Pm = self.E[sc % 2], self.Pm[sc % 2]
                self.act(E, self.bank(3 + sc % 3), AF.Exp)
                self.tt("dve", Pm, E, self.MASKT.idx(sc), ALU.mult)
                for c in range(2):
                    self.mm(self.bank(c), self.CKVtok.idx(sc).cols(c * 128, (c + 1) * 128), Pm, sc == 0, sc == nkc - 1)
                self.mm(self.bank(2), self.ones_bf, Pm, sc == 0, sc == nkc - 1)
            self.act(self.rden, self.bank(2), AF.Ln)
            for c in range(2):
                self.copy("act", self.OLn.idx(c), self.bank(c))
            self.act(self.rden, self.rden, AF.Exp, scale=-1.0)
            bo = self.bank(6)
            for c in range(2):
                self.mm(bo, sUV.view(sUV.ap[:, c, h, :]), self.OLn.idx(c), c == 0, c == 1)
            self.tt("dve", O.idx(h), bo, self.rden, ALU.mult)
        self.ps_rr = 3
        self.whold = None
        for og in range(4):
            slot = self.wget(self.pc[("aout", j, og)])
            for dd in range(4):
                d = og * 4 + dd
                ps = self.next_bank()
                for kc in range(16):
                    self.mm(ps, slot.idx(kc).cols(dd * 128, (dd + 1) * 128), O.idx(kc), kc == 0, kc == 15)
                self.tt("dve", R.idx(d), ps, R.idx(d), ALU.add)


_CACHE = {}


def get_prog(nseq, layers):
    key = (nseq, tuple(layers))
    if key not in _CACHE:
        b = Builder(nseq=nseq, layers=layers)
        _CACHE[key] = b.build()
    return _CACHE[key]


def make_inputs(inputs, nseq, ncores):
    x = np.ascontiguousarray(np.asarray(inputs["x"], np.float32))
    shared = {"vecs": build_vecs(inputs), "consts": build_consts()}
    for name in ("mlp_w1", "mlp_w2", "conv_in", "conv_out", "attn_in", "attn_out"):
        shared[name] = np.ascontiguousarray(np.asarray(inputs[name], np.float32))
    shared["w_uq"] = np.ascontiguousarray(np.asarray(inputs["w_uq"], np.float32)).reshape(2, 512, 2048)
    shared["w_qidx"] = np.ascontiguousarray(np.asarray(inputs["w_qidx"], np.float32)).reshape(2, 512, 1024)
    shared["w_uk"] = np.ascontiguousarray(np.asarray(inputs["w_uk"], np.float32))
    shared["w_uv"] = np.ascontiguousarray(np.asarray(inputs["w_uv"], np.float32))
    in_maps = []
    for c in range(ncores):
        m = dict(shared)
        m["x"] = x[c * nseq:(c + 1) * nseq].reshape(nseq * SEQ, D)
        in_maps.append(m)
    return in_maps


def run(inputs, nseq=2, layers=(0, 1, 2, 3), ncores=NCORES):
    nc = get_prog(nseq, layers)
    in_maps = make_inputs(inputs, nseq, ncores)
    if not layers:
        in_maps = [{k: v for k, v in m.items() if k in ("x", "vecs", "consts")} for m in in_maps]
    elif not any(l % 2 == 1 for l in layers):
        in_maps = [{k: v for k, v in m.items() if not (k.startswith("w_") or k.startswith("attn"))} for m in in_maps]
    res = run_bass_kernel_spmd(nc, in_maps, core_ids=list(range(ncores)))
    out = np.stack([np.asarray(r["y"]).reshape(nseq, SEQ, D) for r in res.results], 0)
    return out.reshape(ncores * nseq, SEQ, D).astype(np.float32)


def kernel(**inputs):
    return run(inputs)
```

```python
from contextlib import ExitStack
import numpy as np
import concourse.bass as bass
import concourse.mybir as mybir
from concourse.bass_utils import run_bass_kernel_spmd

F32 = mybir.dt.float32
BF16 = mybir.dt.bfloat16
U8 = mybir.dt.uint8
ALU = mybir.AluOpType
AF = mybir.ActivationFunctionType
AX = mybir.AxisListType

D = 2048
DFF = 8192
SEQ = 2048
NCORES = 8
TT = 512
EPS = 1e-6
NEG = -1.0e30
SLOT_BYTES = 16384
NSLOT = 3
CELL = 256
SAME_ENG_SYNC = True
ESZ = {F32: 4, BF16: 2, U8: 1}


class T:
    __slots__ = ("ap", "space", "lo", "hi", "esz")

    def __init__(self, ap, space, lo, hi, esz):
        self.ap, self.space, self.lo, self.hi, self.esz = ap, space, lo, hi, esz

    def idx(self, i):
        n = self.ap.shape[1]
        sz = (self.hi - self.lo) // n
        return T(self.ap[:, i], self.space, self.lo + i * sz, self.lo + (i + 1) * sz, self.esz)

    def cols(self, a, b):
        assert len(self.ap.shape) == 2
        return T(self.ap[:, a:b], self.space, self.lo + a * self.esz, self.lo + b * self.esz, self.esz)

    def parts(self, a, b):
        return T(self.ap[a:b], self.space, self.lo, self.hi, self.esz)

    def view(self, ap):
        return T(ap, self.space, self.lo, self.hi, self.esz)


class DR:
    __slots__ = ("ap", "key")

    def __init__(self, ap, key):
        self.ap, self.key = ap, key


class Op:
    __slots__ = ("eng", "fn", "deps", "key", "seq", "sig", "val", "dsem")


class Prog:
    ENGS = ("pe", "act", "dve", "pool", "sp")

    def __init__(self, nc):
        self.nc = nc
        self.lists = {e: [] for e in self.ENGS}
        self.cells = {}
        self.nseq = {e: 0 for e in self.ENGS}
        self.dma_count = {}
        self.out_dma = {}

    def _cells(self, t):
        if isinstance(t, DR):
            return [("dram", t.key)]
        if t.space == "ps":
            return [("ps", c) for c in range(t.lo // 2048, (t.hi + 2047) // 2048)]
        return [("sb", c) for c in range(t.lo // CELL, (t.hi + CELL - 1) // CELL)]

    def add(self, eng, fn, reads=(), writes=(), dsem=None, is_out=False):
        op = Op()
        op.eng, op.fn, op.sig, op.val, op.dsem = eng, fn, False, 0, dsem
        if dsem is not None:
            op.key = ("dma", dsem)
            self.dma_count[dsem] = self.dma_count.get(dsem, 0) + 16
            op.seq = self.dma_count[dsem]
            if is_out:
                self.out_dma[dsem] = op.seq
        else:
            op.key = eng
            self.nseq[eng] += 1
            op.seq = self.nseq[eng]
        deps = {}

        def need(o):
            if o is None or o is op:
                return
            if o.dsem is None and op.dsem is None and o.eng == eng:
                if eng == "pe" or not SAME_ENG_SYNC:
                    return
            cur = deps.get(o.key)
            if cur is None or cur.seq < o.seq:
                deps[o.key] = o

        for t in reads:
            for c in self._cells(t):
                st = self.cells.get(c)
                if st is None:
                    st = self.cells[c] = [None, {}]
                need(st[0])
                cur = st[1].get(op.key)
                if cur is None or cur.seq < op.seq:
                    st[1][op.key] = op
        for t in writes:
            for c in self._cells(t):
                st = self.cells.get(c)
                if st is None:
                    st = self.cells[c] = [None, {}]
                need(st[0])
                for r in st[1].values():
                    need(r)
                st[0] = op
                st[1] = {}
        op.deps = list(deps.values())
        for o in op.deps:
            if o.dsem is None:
                o.sig = True
        self.lists[eng].append(op)
        return op

    def emit(self, stack):
        nc = self.nc
        sems = {e: stack.enter_context(nc.semaphore("sem_" + e)) for e in self.ENGS}
        dsems = {k: stack.enter_context(nc.semaphore("dsem_%s" % str(k))) for k in self.dma_count}
        for e in self.ENGS:
            cnt = 0
            for op in self.lists[e]:
                if op.dsem is None and op.sig:
                    cnt += 1
                    op.val = cnt
        block = stack.enter_context(nc.Block())

        def run(ename, eh):
            known = {}
            for op in self.lists[ename]:
                for o in op.deps:
                    if o.dsem is None:
                        s, v = sems[o.eng], o.val
                    else:
                        s, v = dsems[o.dsem], o.seq
                    if known.get(id(s), 0) >= v:
                        continue
                    known[id(s)] = v
                    eh.wait_ge(s, v)
                ins = op.fn(eh)
                if op.dsem is not None:
                    ins.then_inc(dsems[op.dsem], 16)
                elif op.sig:
                    ins.then_inc(sems[ename], 1)
            if ename == "sp":
                for k, v in self.out_dma.items():
                    eh.wait_ge(dsems[k], v)

        @block.tensor
        def _(e):
            run("pe", e)

        @block.scalar
        def _(e):
            run("act", e)

        @block.vector
        def _(e):
            run("dve", e)

        @block.gpsimd
        def _(e):
            run("pool", e)

        @block.sync
        def _(e):
            run("sp", e)


def vec_layout():
    cols = {}
    n = 0

    def put(name, k):
        nonlocal n
        cols[name] = n
        n += k
    for i in range(4):
        put(("norm_mix", i), 16)
        put(("norm_mlp", i), 16)
    put(("final_norm",), 16)
    for j in range(2):
        for k in range(3):
            put(("conv_w", j, k), 16)
        put(("q_norm", j), 4)
        put(("kv_norm", j), 2)
        put(("ln_g", j), 1)
        put(("ln_b", j), 1)
    return cols, n


def const_layout():
    cols = {}
    n = 0

    def put(name, k):
        nonlocal n
        cols[name] = n
        n += k
    put("ident", 128)
    put("ones", 128)
    for s in range(4):
        put(("cb", s), 512)
    put("m8", 8)
    put("d8", 8)
    put("bsel", 16)
    return cols, n


def build_vecs(inp):
    cols, n = vec_layout()
    v = np.zeros((128, n), np.float32)

    def setv(name, arr):
        arr = np.asarray(arr, np.float32).reshape(-1)
        k = arr.size // 128
        if k == 0:
            v[:arr.size, cols[name]] = arr
        else:
            v[:, cols[name]:cols[name] + k] = arr.reshape(k, 128).T
    for i in range(4):
        setv(("norm_mix", i), inp["norm_mix"][i])
        setv(("norm_mlp", i), inp["norm_mlp"][i])
    setv(("final_norm",), inp["final_norm"])
    for j in range(2):
        for k in range(3):
            setv(("conv_w", j, k), inp["conv_w"][j, k])
        setv(("q_norm", j), inp["q_norm"][j])
        setv(("kv_norm", j), inp["kv_norm"][j])
        setv(("ln_g", j), inp["kidx_ln_g"][j])
        setv(("ln_b", j), inp["kidx_ln_b"][j])
    return v


def build_consts():
    cols, n = const_layout()
    c = np.zeros((128, n), np.float32)
    p = np.arange(128)
    c[:, cols["ident"]:cols["ident"] + 128] = np.eye(128, dtype=np.float32)
    c[:, cols["ones"]:cols["ones"] + 128] = 1.0
    s = np.arange(512)
    for st in range(4):
        vis = s[None, :] <= (st * 128 + p)[:, None]
        c[:, cols[("cb", st)]:cols[("cb", st)] + 512] = np.where(vis, 0.0, NEG)
    c[:, cols["m8"]:cols["m8"] + 8] = (p[:, None] % 8 == np.arange(8)[None, :])
    c[:, cols["d8"]:cols["d8"] + 8] = (p[:, None] // 16 == np.arange(8)[None, :])
    c[:, cols["bsel"]:cols["bsel"] + 16] = (p[:, None] // 8 == np.arange(16)[None, :])
    return c


class Builder:
    def __init__(self, nseq=2, layers=(0, 1, 2, 3), do_final=True, debug=None, ntile=None):
        self.nseq = nseq
        self.ntok = nseq * SEQ
        self.ntile = ntile or self.ntok // TT
        self.layers = tuple(layers)
        self.do_final = do_final
        self.debug = debug
        self.nc = bass.Bass("TRN2", target_bir_lowering=False)
        self.P = Prog(self.nc)
        self.vcols, self.nv = vec_layout()
        self.ccols, self.ncc = const_layout()
        self.sb_off = 0
        self.ps_rr = 0
        self.pieces = []
        self.use_order = []
        self.dsem_n = 0

    def new_dsem(self):
        self.dsem_n += 1
        return self.dsem_n

    def sb(self, nbytes, dtype, shape=None, at=None):
        esz = ESZ[dtype]
        if at is None:
            lo = (self.sb_off + CELL - 1) // CELL * CELL
            self.sb_off = lo + nbytes
            assert self.sb_off <= self.arena_bytes, ("SBUF overflow", self.sb_off)
        else:
            lo = at
        ap = self.arena[:, lo:lo + nbytes].bitcast(dtype)
        if shape is not None and len(shape) == 2:
            ap = ap.rearrange("p (a b) -> p a b", b=shape[1])
        elif shape is not None and len(shape) == 3:
            ap = ap.rearrange("p (a b c) -> p a b c", b=shape[1], c=shape[2])
        return T(ap, "sb", lo, lo + nbytes, esz)

    def bank(self, i, dtype=F32):
        ap = self.psum[:, i * 512:(i + 1) * 512]
        if dtype != F32:
            ap = ap.bitcast(dtype)
        return T(ap, "ps", i * 2048, (i + 1) * 2048, ESZ[dtype])

    def next_bank(self, dtype=F32):
        b = self.bank(self.ps_rr % 8, dtype)
        self.ps_rr += 1
        return b

    def mm(self, out, lhsT, rhs, start, stop):
        self.P.add("pe", lambda e: e.matmul(out.ap, lhsT.ap, rhs.ap, start=start, stop=stop),
                   reads=[lhsT, rhs], writes=[out])

    def tr(self, out, in_, ident):
        self.P.add("pe", lambda e: e.transpose(out.ap, in_.ap, ident.ap), reads=[in_, ident], writes=[out])

    def act(self, out, in_, func, scale=1.0, bias=0.0, extra_reads=()):
        self.P.add("act", lambda e: e.activation(out.ap, in_.ap, func, bias=bias, scale=scale),
                   reads=[in_] + list(extra_reads), writes=[out])

    def tt(self, eng, out, a, b, op):
        h = "dve" if eng == "dve" else "pool"
        self.P.add(h, lambda e: e.tensor_tensor(out.ap, a.ap, b.ap, op), reads=[a, b], writes=[out])

    def ts(self, eng, out, a, s1, s2, op0, op1=None, reads=(), accum=None):
        kw = {}
        if op1 is not None:
            kw["op1"] = op1
        w = [out]
        if accum is not None:
            kw["accum_out"] = accum.ap
            w.append(accum)
        self.P.add(eng, lambda e: e.tensor_scalar(out.ap, a.ap, s1, s2, op0, **kw),
                   reads=[a] + list(reads), writes=w)

    def stt(self, out, a, scalar, b, op0, op1, reads=()):
        self.P.add("dve", lambda e: e.scalar_tensor_tensor(out.ap, a.ap, scalar, b.ap, op0, op1),
                   reads=[a, b] + list(reads), writes=[out])

    def copy(self, eng, out, in_):
        if eng == "act":
            self.P.add("act", lambda e: e.copy(out.ap, in_.ap), reads=[in_], writes=[out])
        else:
            self.P.add(eng, lambda e: e.tensor_copy(out.ap, in_.ap), reads=[in_], writes=[out])

    def dma(self, q, out, in_, dsem, reads, writes, is_out=False):
        self.P.add(q, lambda e: e.dma_start(out=out, in_=in_), reads=reads, writes=writes, dsem=dsem,
                   is_out=is_out)

    def vcol(self, name, c=0, n=1, parts=128):
        o = self.vcols[name] + c
        return self.vecs.ap[0:parts, o:o + n]

    def add_piece(self, src_ap, shape):
        n = int(np.prod(shape[1:]))
        assert n * 2 <= SLOT_BYTES
        idx = len(self.pieces)
        scr = self.nc.dram_tensor("wscr%d" % idx, [128, n], BF16, kind="Internal").ap()
        res = DR(scr, ("w", idx))
        sem = 100 + 2 * self.cast_group + (idx % 2)
        if len(shape) == 3:
            dst3 = scr.rearrange("p (a b) -> p a b", b=shape[2])
            self.cast_thunks[-1].append(lambda ex: self.dma("pool", dst3, src_ap, sem, reads=ex, writes=[res]))
        else:
            sub = shape[2] * shape[3]
            for g in range(shape[1]):
                dst = scr[:, g * sub:(g + 1) * sub].rearrange("p (a b) -> p a b", b=shape[3])
                self.cast_thunks[-1].append(lambda ex, dst=dst, g=g: self.dma(
                    "pool", dst, src_ap[g], sem, reads=ex, writes=[DR(None, ("w", idx, g))]))
        self.pieces.append((res, n, shape))
        return idx

    def wget(self, idx, hold=False):
        pos = self.wpos
        assert self.use_order[pos] == idx, (pos, idx, self.use_order[pos])
        if hold and self.whold is None:
            self.whold = pos
        base = pos if self.whold is None else self.whold
        while self.wissued < min(len(self.use_order), base + NSLOT):
            k = self.wissued
            res, n, shape = self.pieces[self.use_order[k]]
            s = k % NSLOT
            st = self.slots[s]
            dst = st.ap[:, 0:n]
            rr = [res] if len(shape) == 3 else [DR(None, ("w", self.use_order[k], g)) for g in range(shape[1])]
            self.dma("sp", dst, res.ap, 200 + s, reads=rr, writes=[T(dst, "sb", st.lo, st.lo + 2 * n, 2)])
            self.wissued += 1
        self.wpos += 1
        res, n, shape = self.pieces[idx]
        st = self.slots[pos % NSLOT]
        ap = st.ap[:, 0:n]
        if len(shape) == 3:
            ap = ap.rearrange("p (a b) -> p a b", b=shape[2])
        elif len(shape) == 4:
            ap = ap.rearrange("p (a b c) -> p a b c", b=shape[2], c=shape[3])
        return T(ap, "sb", st.lo, st.lo + 2 * n, 2)

    def rmsnorm(self, src, nch, gname, inv_n, dst, ones_parts=128, width=TT):
        ps = self.next_bank()
        for c in range(nch):
            sq = self.sqt[c % 2]
            self.act(sq, src.idx(c), AF.Square)
            self.mm(ps, self.ones_bf, sq, c == 0, c == nch - 1)
        self.act(self.rstd, ps, AF.Ln, scale=inv_n, bias=self.eps_t.ap, extra_reads=[self.eps_t])
        self.act(self.rstd, self.rstd, AF.Exp, scale=-0.5)
        for c in range(nch):
            self.stt(dst.idx(c), src.idx(c), self.vcol(gname, c), self.rstd, ALU.mult, ALU.mult,
                     reads=[self.vecs])

    def mlp(self, layer):
        R, xn, h = self.R, self.xn, self.hbuf
        self.rmsnorm(R, 16, ("norm_mlp", layer), 1.0 / D, xn)
        for hh in range(2):
            for fg in range(hh * 8, hh * 8 + 8):
                slot = self.wget(self.pc[("w1", layer, fg)])
                for jj in range(4):
                    j = (fg - hh * 8) * 4 + jj
                    ps = self.next_bank()
                    for kc in range(16):
                        self.mm(ps, slot.idx(kc).cols(jj * 128, (jj + 1) * 128), xn.idx(kc), kc == 0, kc == 15)
                    tmp = self.rtmp[j % 2]
                    self.act(tmp, ps, AF.Relu)
                    self.tt("dve", h.idx(j), tmp, tmp, ALU.mult)
            for half in range(2):
                banks = [self.bank(i) for i in range(8)]
                for kg in range(hh * 4, hh * 4 + 4):
                    slot = self.wget(self.pc[("w2", layer, half, kg)])
                    for kk in range(8):
                        k = (kg - hh * 4) * 8 + kk
                        for dl in range(8):
                            self.mm(banks[dl], slot.idx(kk).cols(dl * 128, (dl + 1) * 128), h.idx(k), k == 0, k == 31)
                for dl in range(8):
                    d = half * 8 + dl
                    self.tt("dve", R.idx(d), banks[dl], R.idx(d), ALU.add)
        self.ps_rr = 0

    def conv_mixer(self, layer, first_in_seq):
        j = layer // 2
        R, xn = self.R, self.xn
        u = self.sb(16 * TT * 2, BF16, [16, TT], at=self.hbuf.lo)
        self.rmsnorm(R, 16, ("norm_mix", layer), 1.0 / D, xn)
        for dj in range(16):
            slot = self.wget(self.pc[("cin", j, dj)])
            bks = [self.next_bank() for _ in range(3)]
            for g in range(3):
                for kc in range(16):
                    lw = slot.view(slot.ap[:, g, kc, :])
                    self.mm(bks[g], lw, xn.idx(kc), kc == 0, kc == 15)
            z = self.zt[dj % 2]
            cgs = self.cgs[dj % 2]
            acc = self.acc[dj % 2]
            halo = self.halo.idx(dj)
            self.copy("act", cgs, bks[1])
            if first_in_seq:
                self.P.add("pool", lambda e, z=z: e.memset(z.ap[:, 0:2], 0.0), writes=[z.cols(0, 2)])
            else:
                self.copy("pool", z.cols(0, 2), halo)
            self.tt("dve", z.cols(2, TT + 2), bks[2], cgs, ALU.mult)
            self.ts("dve", acc, z.cols(2, TT + 2), self.vcol(("conv_w", j, 2), dj), None, ALU.mult,
                    reads=[self.vecs])
            self.stt(acc, z.cols(1, TT + 1), self.vcol(("conv_w", j, 1), dj), acc, ALU.mult, ALU.add,
                     reads=[self.vecs])
            self.stt(acc, z.cols(0, TT), self.vcol(("conv_w", j, 0), dj), acc, ALU.mult, ALU.add,
                     reads=[self.vecs])
            self.copy("pool", halo, z.cols(TT, TT + 2))
            self.tt("dve", u.idx(dj), bks[0], acc, ALU.mult)
        for og in range(4):
            slot = self.wget(self.pc[("cout", j, og)])
            for dd in range(4):
                d = og * 4 + dd
                ps = self.next_bank()
                for kc in range(16):
                    self.mm(ps, slot.idx(kc).cols(dd * 128, (dd + 1) * 128), u.idx(kc), kc == 0, kc == 15)
                self.tt("dve", R.idx(d), ps, R.idx(d), ALU.add)

    def load_x_tile(self, tile):
        xin = self.sb(4 * D * 4, F32, [4, D], at=self.hbuf.lo)
        src = self.x[tile * TT:(tile + 1) * TT, :].rearrange("(a p) d -> p a d", p=128)
        self.dma("sp", xin.ap, src, 1, reads=[], writes=[xin])
        for d in range(16):
            ps = self.next_bank()
            for tc in range(4):
                self.tr(ps.cols(tc * 128, (tc + 1) * 128), xin.idx(tc).cols(d * 128, (d + 1) * 128), self.ident_f)
            self.copy("act" if d % 2 else "dve", self.R.idx(d), ps)

    def final_out(self, tile):
        R = self.R
        xf = self.sb(16 * TT * 4, F32, [16, TT], at=self.hbuf.lo)
        self.rmsnorm(R, 16, ("final_norm",), 1.0 / D, xf)
        for tc in range(4):
            ot = self.otile[tc % 2]
            for dg in range(4):
                ps = self.next_bank()
                for dd in range(4):
                    self.tr(ps.cols(dd * 128, (dd + 1) * 128), xf.idx(dg * 4 + dd).cols(tc * 128, (tc + 1) * 128),
                            self.ident_f)
                self.copy("act" if dg % 2 else "dve", ot.cols(dg * 512, (dg + 1) * 512), ps)
            r0 = tile * TT + tc * 128
            self.dma("sp", self.y[r0:r0 + 128, :], ot.ap, 10 + tc % 2, reads=[ot], writes=[], is_out=True)

    def emit_casts(self, group, part, nparts, anchor):
        th = self.cast_thunks[group]
        n = len(th)
        n0 = len(self.P.lists["pool"])
        for f in th[part * n // nparts:(part + 1) * n // nparts]:
            f([])
        new_ops = self.P.lists["pool"][n0:]
        if anchor is not None and new_ops:
            new_ops[0].deps.append(anchor)
            anchor.sig = True
        self.cast_ops.setdefault(group, []).extend(new_ops)
        if part == nparts - 1:
            for op in self.cast_ops[group]:
                if op.dsem is not None and op.dsem >= 100:
                    op.seq = self.P.dma_count[op.dsem]

    def store_R(self, tile):
        dst = self.rs[:, :, tile * TT:(tile + 1) * TT].rearrange("c p t -> p c t")
        self.dma("sp", dst, self.R.ap, 2, reads=[self.R], writes=[DR(None, ("rs", tile))])

    def load_R(self, tile):
        src = self.rs[:, :, tile * TT:(tile + 1) * TT].rearrange("c p t -> p c t")
        self.dma("sp", self.R.ap, src, 3, reads=[DR(None, ("rs", tile))], writes=[self.R])

    def build(self):
        nc = self.nc
        st = ExitStack()
        self.stack = st
        ntok = self.ntok
        self.x = nc.dram_tensor("x", [ntok, D], F32, kind="ExternalInput").ap()
        self.y = nc.dram_tensor("y", [ntok, D], F32, kind="ExternalOutput").ap()
        vecs_d = nc.dram_tensor("vecs", [128, self.nv], F32, kind="ExternalInput").ap()
        consts_d = nc.dram_tensor("consts", [128, self.ncc], F32, kind="ExternalInput").ap()
        W = {}
        for name, shape in (("mlp_w1", [4, D, DFF]), ("mlp_w2", [4, DFF, D]), ("conv_in", [2, D, 3 * D]),
                            ("conv_out", [2, D, D]), ("attn_in", [2, D, 848]), ("w_uq", [2, 512, 2048]),
                            ("w_uk", [2, 16, 128, 256]), ("w_uv", [2, 16, 256, 128]),
                            ("w_qidx", [2, 512, 1024]), ("attn_out", [2, D, D])):
            if name.startswith("mlp") and not self.layers:
                continue
            if name.startswith("conv") and not any(l % 2 == 0 for l in self.layers):
                continue
            if (name.startswith("w_") or name.startswith("attn")) and not any(l % 2 == 1 for l in self.layers):
                continue
            W[name] = nc.dram_tensor(name, shape, F32, kind="ExternalInput").ap()
        self.W = W
        self.rs = nc.dram_tensor("rs", [16, 128, ntok], F32, kind="Internal").ap()

        self.arena_bytes = 212000
        arena_t = st.enter_context(nc.sbuf_tensor("arena", [128, self.arena_bytes], U8))
        self.arena = arena_t.ap() if hasattr(arena_t, "ap") else arena_t[:]
        psum_t = st.enter_context(nc.psum_tensor("psum", [128, 4096], F32))
        self.psum = psum_t.ap() if hasattr(psum_t, "ap") else psum_t[:]

        self.vecs = self.sb(self.nv * 4, F32)
        self.cst = self.sb(self.ncc * 4, F32)
        self.ident_f = self.cst.cols(self.ccols["ident"], self.ccols["ident"] + 128)
        self.ident_bf = self.sb(256, BF16)
        self.ones_bf = self.sb(256, BF16)
        self.eps_t = self.sb(4, F32)
        self.slots = [self.sb(SLOT_BYTES, BF16) for _ in range(NSLOT)]
        self.R = self.sb(16 * TT * 4, F32, [16, TT])
        self.xn = self.sb(16 * TT * 2, BF16, [16, TT])
        self.hbuf = self.sb(32 * TT * 2, BF16, [32, TT])
        self.sqt = [self.sb(TT * 2, BF16) for _ in range(2)]
        self.rtmp = [self.sb(TT * 2, BF16) for _ in range(2)]
        self.rstd = self.sb(TT * 4, F32)
        self.halo = self.sb(16 * 2 * 4, F32, [16, 2])
        o = self.hbuf.lo + 16 * TT * 2
        self.zt = [self.sb(2112, F32, at=o + i * 2112)for i in range(2)]
        self.zt = [T(z.ap[:, 0:TT + 2], "sb", z.lo, z.hi, 4) for z in self.zt]
        o += 2 * 2112
        self.cgs = [self.sb(TT * 4, F32, at=o + i * TT * 4) for i in range(2)]
        o += 2 * TT * 4
        self.acc = [self.sb(TT * 4, F32, at=o + i * TT * 4) for i in range(2)]
        self.otile = [self.sb(D * 4, F32, at=self.xn.lo + i * D * 4) for i in range(2)]
        self.attn_alloc()

        self.dma("sp", self.vecs.ap, vecs_d, 20, reads=[], writes=[self.vecs])
        self.dma("sp", self.cst.ap, consts_d, 21, reads=[], writes=[self.cst])
        self.copy("dve", self.ident_bf, self.ident_f)
        self.copy("dve", self.ones_bf, self.cst.cols(self.ccols["ones"], self.ccols["ones"] + 128))
        self.P.add("dve", lambda e: e.memset(self.eps_t.ap, EPS), writes=[self.eps_t])

        self.pc = {}
        order = []
        self.cast_thunks = []
        for layer in self.layers:
            j = layer // 2
            per_tile = []
            self.cast_group = len(self.cast_thunks)
            self.cast_thunks.append([])
            if layer % 2 == 0:
                for dj in range(16):
                    src = [W["conv_in"][j][:, g * D + dj * 128:g * D + (dj + 1) * 128].rearrange(
                        "(k p) c -> p k c", p=128) for g in range(3)]
                    self.pc[("cin", j, dj)] = self.add_piece(src, [128, 3, 16, 128])
                    per_tile.append(self.pc[("cin", j, dj)])
                for og in range(4):
                    src = W["conv_out"][j][:, og * 512:(og + 1) * 512].rearrange("(k p) c -> p k c", p=128)
                    self.pc[("cout", j, og)] = self.add_piece(src, [128, 16, 512])
                    per_tile.append(self.pc[("cout", j, og)])
            else:
                per_tile += self.attn_pieces(j)
            for hh in range(2):
                for fg in range(hh * 8, hh * 8 + 8):
                    src = W["mlp_w1"][layer][:, fg * 512:(fg + 1) * 512].rearrange("(k p) c -> p k c", p=128)
                    self.pc[("w1", layer, fg)] = self.add_piece(src, [128, 16, 512])
                    per_tile.append(self.pc[("w1", layer, fg)])
                for half in range(2):
                    for kg in range(hh * 4, hh * 4 + 4):
                        src = W["mlp_w2"][layer][kg * 1024:(kg + 1) * 1024,
                                                 half * 1024:(half + 1) * 1024].rearrange("(k p) c -> p k c", p=128)
                        self.pc[("w2", layer, half, kg)] = self.add_piece(src, [128, 8, 1024])
                        per_tile.append(self.pc[("w2", layer, half, kg)])
            order += per_tile * self.ntile
        self.use_order = order
        self.wpos = 0
        self.wissued = 0
        self.whold = None
        self.cast_ops = {}

        nl = len(self.layers)
        if nl:
            self.emit_casts(0, 0, 1, None)
        for li, layer in enumerate(self.layers):
            for tile in range(self.ntile):
                if li + 1 < nl:
                    self.emit_casts(li + 1, tile, self.ntile, self.P.lists["act"][-1] if tile else None)
                if li == 0:
                    self.load_x_tile(tile)
                else:
                    self.load_R(tile)
                if layer % 2 == 0:
                    self.conv_mixer(layer, tile % (SEQ // TT) == 0)
                else:
                    self.attn_mixer(layer, tile)
                if self.debug == "attn_only":
                    continue
                self.mlp(layer)
                if li == nl - 1:
                    self.final_out(tile)
                else:
                    self.store_R(tile)
        if nl == 0:
            for tile in range(self.ntile):
                self.load_x_tile(tile)
                self.final_out(tile)
        self.P.emit(st)
        st.close()
        return nc

    def attn_alloc(self):
        if not any(l % 2 == 1 for l in self.layers):
            return
        self.CKVT = self.sb(2 * SEQ * 2, BF16, [2, SEQ])
        self.CKVtok = self.sb(16 * 256 * 2, BF16, [16, 256])
        self.KI = self.sb(SEQ * 2, BF16)
        self.CQ = self.sb(4 * TT * 2, BF16, [4, TT])
        self.MASKT = self.sb(16 * TT * 2, BF16, [16, TT])
        self.thr_rep = self.sb(TT * 4, F32)
        self.vecs_a = [self.sb(8 * 4, F32) for _ in range(4)]
        self.wtok = self.sb(4 * 16 * 4, F32, [4, 16])
        self.wcol = self.sb(16 * 4, F32)
        self.rel = [self.sb(TT * 2, BF16) for _ in range(3)]
        base = (self.sb_off + CELL - 1) // CELL * CELL
        self.cq_raw = self.sb(4 * TT * 4, F32, [4, TT])
        self.kv_raw = self.sb(2 * TT * 4, F32, [2, TT])
        self.ki_raw = self.sb(TT * 4, F32)
        o = base
        self.junk = self.sb(SEQ * 2, BF16, at=o); o += SEQ * 2
        self.Amat = self.sb(128 * 4, F32, [8, 16], at=o); o += 512
        self.dg = self.sb(128 * 4, F32, at=o); o += 512
        self.tmpdiag = self.sb(TT * 4, F32, at=o); o += TT * 4
        o = base
        self.QH = self.sb(TT * 2, BF16, at=o); o += TT * 2
        self.QL = self.sb(2 * TT * 2, BF16, [2, TT], at=o); o += 2 * TT * 2
        self.E = [self.sb(TT * 2, BF16, at=o + i * TT * 2) for i in range(2)]; o += 2 * TT * 2
        self.Pm = [self.sb(TT * 2, BF16, at=o + i * TT * 2) for i in range(2)]; o += 2 * TT * 2
        self.rden = self.sb(TT * 4, F32, at=o); o += TT * 4
        self.OLn = self.sb(2 * TT * 2, BF16, [2, TT], at=o); o += 2 * TT * 2
        assert o <= self.sb_off
        o = self.hbuf.lo
        self.SC = [self.sb(SEQ * 4, F32, at=o + i * SEQ * 4) for i in range(2)]; o += 2 * SEQ * 4
        self.Qi = [self.sb(128 * 16 * 2, BF16, [128, 16], at=o + i * 4096) for i in range(2)]; o += 8192
        self.Z = [self.sb(16 * 128 * 2, BF16, [16, 128], at=o + i * 4096) for i in range(2)]; o += 8192
        assert o <= self.hbuf.hi

    def attn_pieces(self, j):
        W = self.W
        lst = []

        def reg(key, src, shape):
            self.pc[key] = self.add_piece(src, shape)
            lst.append(self.pc[key])
        reg(("ainA", j), W["attn_in"][j][:, 0:512].rearrange("(k p) c -> p k c", p=128), [128, 16, 512])
        reg(("ainB", j), W["attn_in"][j][:, 512:848].rearrange("(k p) c -> p k c", p=128), [128, 16, 336])
        reg(("wqidx", j), W["w_qidx"][j].rearrange("(k p) c -> p k c", p=128), [128, 4, 1024])
        reg(("wuq", j), W["w_uq"][j].rearrange("(k p) c -> p k c", p=128), [128, 4, 2048])
        reg(("wuk", j), W["w_uk"][j].rearrange("h d c -> d h c"), [128, 16, 256])
        reg(("wuv", j), [W["w_uv"][j][:, cc * 128:(cc + 1) * 128, :].rearrange("h p v -> p h v") for cc in range(2)],
            [128, 2, 16, 128])
        for og in range(4):
            reg(("aout", j, og), W["attn_out"][j][:, og * 512:(og + 1) * 512].rearrange("(k p) c -> p k c", p=128),
                [128, 16, 512])
        return lst

    def rmsnorm_l(self, srcs, gname, inv_n, dsts):
        ps = self.next_bank()
        n = len(srcs)
        for c in range(n):
            sq = self.sqt[c % 2]
            self.act(sq, srcs[c], AF.Square)
            self.mm(ps, self.ones_bf, sq, c == 0, c == n - 1)
        self.act(self.rstd, ps, AF.Ln, scale=inv_n, bias=self.eps_t.ap, extra_reads=[self.eps_t])
        self.act(self.rstd, self.rstd, AF.Exp, scale=-0.5)
        for c in range(n):
            self.stt(dsts[c], srcs[c], self.vcol(gname, c), self.rstd, ALU.mult, ALU.mult, reads=[self.vecs])

    def attn_mixer(self, layer, tile):
        j = layer // 2
        qt = tile % (SEQ // TT)
        t0 = qt * TT
        nkc = 4 * (qt + 1)
        nk = nkc * 128
        R, xn = self.R, self.xn
        cc_ = self.ccols
        ones_f = self.cst.cols(cc_["ones"], cc_["ones"] + 128)
        NIT = 20
        self.rmsnorm(R, 16, ("norm_mix", layer), 1.0 / D, xn)
        slotA = self.wget(self.pc[("ainA", j)])
        for m in range(4):
            ps = self.next_bank()
            for kc in range(16):
                self.mm(ps, slotA.idx(kc).cols(m * 128, (m + 1) * 128), xn.idx(kc), kc == 0, kc == 15)
            self.copy("act" if m % 2 else "dve", self.cq_raw.idx(m), ps)
        slotB = self.wget(self.pc[("ainB", j)])
        for m in range(2):
            ps = self.next_bank()
            for kc in range(16):
                self.mm(ps, slotB.idx(kc).cols(m * 128, (m + 1) * 128), xn.idx(kc), kc == 0, kc == 15)
            self.copy("act" if m % 2 else "dve", self.kv_raw.idx(m), ps)
        ps = self.next_bank()
        for kc in range(16):
            self.mm(ps.parts(0, 64), slotB.idx(kc).cols(256, 320), xn.idx(kc), kc == 0, kc == 15)
        kir = self.ki_raw.parts(0, 64)
        self.copy("act", kir, ps.parts(0, 64))
        ps = self.next_bank()
        for tc in range(4):
            for kc in range(16):
                self.mm(ps.cols(tc * 16, (tc + 1) * 16), xn.idx(kc).cols(tc * 128, (tc + 1) * 128),
                        slotB.idx(kc).cols(320, 336), kc == 0, kc == 15)
        self.act(self.wtok.view(self.wtok.ap.rearrange("p a b -> p (a b)")), ps.cols(0, 64), AF.Identity,
                 scale=1.0 / 32.0)
        self.rmsnorm_l([self.cq_raw.idx(c) for c in range(4)], ("q_norm", j), 1.0 / 512, [self.CQ.idx(c) for c in range(4)])
        kvd = [self.CKVT.idx(c).cols(t0, t0 + TT) for c in range(2)]
        self.rmsnorm_l([self.kv_raw.idx(c) for c in range(2)], ("kv_norm", j), 1.0 / 256, kvd)
        mu = self.cq_raw.idx(0).parts(0, 64)
        var = self.cq_raw.idx(1).parts(0, 64)
        xc = self.cq_raw.idx(2).parts(0, 64)
        sqk = self.cq_raw.idx(3).parts(0, 64)
        on64 = ones_f.parts(0, 64).cols(0, 64)
        self.act(sqk, kir, AF.Square)
        pm = self.next_bank().parts(0, 64)
        self.mm(pm, on64, kir, True, True)
        pv = self.next_bank().parts(0, 64)
        self.mm(pv, on64, sqk, True, True)
        self.act(mu, pm, AF.Identity, scale=1.0 / 64)
        self.tt("dve", var, mu, mu, ALU.mult)
        self.stt(var, pv, 1.0 / 64, var, ALU.mult, ALU.subtract)
        self.act(var, var, AF.Ln, bias=self.eps_t.ap[0:64], extra_reads=[self.eps_t])
        self.act(var, var, AF.Exp, scale=-0.5)
        self.tt("dve", xc, kir, mu, ALU.subtract)
        self.tt("dve", xc, xc, var, ALU.mult)
        kid = self.KI.cols(t0, t0 + TT).parts(0, 64)
        self.ts("dve", kid, xc, self.vcol(("ln_g", j), 0, 1, 64), self.vcol(("ln_b", j), 0, 1, 64), ALU.mult, ALU.add,
                reads=[self.vecs])
        pb = self.next_bank(BF16)
        for i in range(4):
            for c in range(2):
                self.tr(pb.cols(i * 256 + c * 128, i * 256 + (c + 1) * 128),
                        self.CKVT.idx(c).cols(t0 + i * 128, t0 + (i + 1) * 128), self.ident_bf)
        dstk = self.CKVtok.view(self.CKVtok.ap[:, 4 * qt:4 * qt + 4, :].rearrange("p a b -> p (a b)"))
        dstk.lo = self.CKVtok.lo + 4 * qt * 512
        dstk.hi = dstk.lo + 4 * 512
        self.copy("dve", dstk, pb)
        slotQ = self.wget(self.pc[("wqidx", j)])
        NITB = 13

        def S_steps(st):
            Qi, Z, SC = self.Qi[st % 2], self.Z[st % 2], self.SC[st % 2]
            steps = []

            def qi_group(hg):
                ps = self.bank(6 + hg % 2)
                for hh in range(4):
                    h = hg * 4 + hh
                    for kc in range(4):
                        self.mm(ps.parts(0, 64).cols(hh * 128, (hh + 1) * 128), slotQ.idx(kc).cols(h * 64, (h + 1) * 64),
                                self.CQ.idx(kc).cols(st * 128, (st + 1) * 128), kc == 0, kc == 3)
                dq = Qi.view(Qi.ap[0:64, :, hg * 4:hg * 4 + 4].rearrange("p t h -> p h t"))
                sq_ = ps.view(ps.ap[0:64, :].rearrange("p (h t) -> p h t", t=128))
                self.copy("act" if hg % 2 else "dve", dq, sq_)
            for hg in range(4):
                steps.append(lambda hg=hg: qi_group(hg))

            def wsel_a():
                wt = self.wtok.idx(st)
                for tp in range(8):
                    self.ts("dve", self.Amat.idx(tp), wt, self.cst.ap[:, cc_["m8"] + tp:cc_["m8"] + tp + 1], None,
                            ALU.mult, reads=[self.cst])
                pw = self.bank(6).cols(0, 16)
                self.mm(pw, self.Amat.view(self.Amat.ap.rearrange("p a b -> p (a b)")),
                        self.cst.cols(cc_["bsel"], cc_["bsel"] + 16), True, True)
                self.copy("dve", self.wcol, pw)
                self.P.add("pool", lambda e, Z=Z: e.memset(Z.ap, 0.0), writes=[Z])

            def wsel_b(g0):
                d8 = self.cst.cols(cc_["d8"], cc_["d8"] + 8)
                for g in range(g0, g0 + 8):
                    zg = Z.idx(g).cols(8 * g, 8 * g + 8)
                    self.ts("dve", zg, d8, self.wcol.ap[:, g:g + 1], None, ALU.mult, reads=[self.wcol])
            steps.append(wsel_a)
            steps.append(lambda: wsel_b(0))
            steps.append(lambda: wsel_b(8))
            for sb_ in range(qt + 1):
                scb = self.bank(4 + (st * 4 + sb_) % 2)
                kis = self.KI.cols(sb_ * TT, (sb_ + 1) * TT).parts(0, 64)

                def s1(g, kis=kis):
                    lq = Qi.view(Qi.ap[0:64, 8 * g:8 * g + 8, :].rearrange("p t h -> p (t h)"))
                    self.mm(self.bank(g % 4), lq, kis, True, True)

                def gstep(g, scb=scb, sb_=sb_, s1=s1):
                    if g == 0:
                        s1(0)
                        s1(1)
                    if g + 2 < 16:
                        s1(g + 2)
                    rel = self.rel[g % 3]
                    self.act(rel, self.bank(g % 4), AF.Relu)
                    self.mm(scb, Z.idx(g), rel, g == 0, g == 15)
                    if g == 15:
                        scd = SC.cols(sb_ * TT, (sb_ + 1) * TT)
                        if sb_ == qt:
                            self.tt("dve", scd, scb, self.cst.cols(cc_[("cb", st)], cc_[("cb", st)] + TT), ALU.add)
                        else:
                            self.copy("act", scd, scb)
                for g in range(16):
                    steps.append(lambda g=g, gstep=gstep: gstep(g))
            return steps

        def B_steps(st):
            SC = self.SC[st % 2]
            va = self.vecs_a[st]
            v_hi, v_lo, v_w0, v_mid, v_cnt, v_gew, v_t = [va.cols(i, i + 1) for i in range(7)]
            steps = []
            if qt == 0 and st < 2:
                steps.append(lambda: self.P.add("dve", lambda e, v=v_lo: e.memset(v.ap, -1.0e29), writes=[v_lo]))
                return steps
            scv = SC.cols(0, nk)

            def init():
                self.P.add("dve", lambda e, o=v_hi, i=scv: e.tensor_reduce(o.ap, i.ap, AX.X, ALU.max),
                           reads=[scv], writes=[v_hi])
                cbt = self.cst.cols(cc_[("cb", st)], cc_[("cb", st)] + TT)
                self.stt(self.tmpdiag, cbt, -2.0, SC.cols(qt * TT, nk), ALU.mult, ALU.add)
                self.P.add("dve", lambda e, o=v_lo, i=self.tmpdiag: e.tensor_reduce(o.ap, i.ap, AX.X, ALU.min),
                           reads=[self.tmpdiag], writes=[v_lo])
                if qt > 0:
                    scf = SC.cols(0, qt * TT)
                    self.P.add("dve", lambda e, o=v_t, i=scf: e.tensor_reduce(o.ap, i.ap, AX.X, ALU.min),
                               reads=[scf], writes=[v_t])
                    self.tt("dve", v_lo, v_lo, v_t, ALU.min)
                self.tt("dve", v_w0, v_hi, v_lo, ALU.subtract)
            steps.append(init)
            jk = self.junk.cols(0, nk)

            def it(k):
                f = 2.0 ** -(k + 1)
                self.stt(v_mid, v_w0, f, v_lo, ALU.mult, ALU.add)
                self.P.add("dve", lambda e, o=jk, i=scv, m=v_mid, c=v_cnt: e.tensor_scalar(
                    o.ap, i.ap, m.ap, None, ALU.is_ge, op1=ALU.add, accum_out=c.ap),
                    reads=[scv, v_mid, jk], writes=[v_cnt])
                self.ts("dve", v_gew, v_cnt, 255.5, f, ALU.is_ge, ALU.mult)
                self.stt(v_lo, v_gew, v_w0.ap, v_lo, ALU.mult, ALU.add, reads=[v_w0])
            for k in range(NITB):
                steps.append(lambda k=k: it(k))
            return steps

        def M_steps(st):
            SC = self.SC[st % 2]
            v_lo = self.vecs_a[st].cols(1, 2)
            self.ts("dve", self.dg, self.ident_f, v_lo.ap, None, ALU.mult, reads=[v_lo])
            pt = self.bank(7)
            for r in range(4):
                self.mm(pt.cols(r * 128, (r + 1) * 128), ones_f, self.dg, True, True)
            self.copy("act", self.thr_rep, pt)
            for scg in range(qt + 1):
                pT = self.bank(6 + scg % 2)
                for i in range(4):
                    sc = scg * 4 + i
                    self.tr(pT.cols(i * 128, (i + 1) * 128), SC.cols(sc * 128, (sc + 1) * 128), self.ident_f)
                mo = self.MASKT.view(self.MASKT.ap[:, scg * 4:scg * 4 + 4, st * 128:(st + 1) * 128])
                mo.lo = self.MASKT.lo + scg * 4 * TT * 2
                mo.hi = mo.lo + 4 * TT * 2
                pin = pT.view(pT.ap.rearrange("p (a b) -> p a b", b=128))
                tin = self.thr_rep.view(self.thr_rep.ap.rearrange("p (a b) -> p a b", b=128))
                self.tt("dve", mo, pin, tin, ALU.is_ge)

        def merge(a, b):
            na, nb = len(a), len(b)
            ia = ib = 0
            while ia < na or ib < nb:
                if ia < na:
                    a[ia]()
                    ia += 1
                while ib < nb and (ia >= na or ib * na < ia * nb):
                    b[ib]()
                    ib += 1

        merge(S_steps(0), [])
        for st in range(1, 4):
            merge(S_steps(st), B_steps(st - 1))
            M_steps(st - 1)
        merge(B_steps(3), [])
        M_steps(3)
        sUQ = self.wget(self.pc[("wuq", j)], hold=True)
        sUK = self.wget(self.pc[("wuk", j)])
        sUV = self.wget(self.pc[("wuv", j)])
        O = xn
        scale = 128.0 ** -0.5
        for h in range(16):
            b6 = self.bank(6)
            for kc in range(4):
                self.mm(b6, sUQ.idx(kc).cols(h * 128, (h + 1) * 128), self.CQ.idx(kc), kc == 0, kc == 3)
            self.copy("act", self.QH, b6)
            for c in range(2):
                bq = self.bank(7 - c)
                self.mm(bq, sUK.idx(h).cols(c * 128, (c + 1) * 128), self.QH, True, True)
                self.act(self.QL.idx(c), bq, AF.Identity, scale=scale)

            def qk(sc):
                bs = self.bank(3 + sc % 3)
                for c in range(2):
                    self.mm(bs, self.CKVT.idx(c).cols(sc * 128, (sc + 1) * 128), self.QL.idx(c), c == 0, c == 1)
            qk(0)
            if nkc > 1:
                qk(1)
            for sc in range(nkc):
                if sc + 2 < nkc:
                    qk(sc + 2)
                E, Pm = self.E[sc % 2], self.Pm[sc % 2]
                self.act(E, self.bank(3 + sc % 3), AF.Exp)
                self.tt("dve", Pm, E, self.MASKT.idx(sc), ALU.mult)
                for c in range(2):
                    self.mm(self.bank(c), self.CKVtok.idx(sc).cols(c * 128, (c + 1) * 128), Pm, sc == 0, sc == nkc - 1)
                self.mm(self.bank(2), self.ones_bf, Pm, sc == 0, sc == nkc - 1)
            self.act(self.rden, self.bank(2), AF.Ln)
            for c in range(2):
                self.copy("act", self.OLn.idx(c), self.bank(c))
            self.act(self.rden, self.rden, AF.Exp, scale=-1.0)
            bo = self.bank(6)
            for c in range(2):
                self.mm(bo, sUV.view(sUV.ap[:, c, h, :]), self.OLn.idx(c), c == 0, c == 1)
            self.tt("dve", O.idx(h), bo, self.rden, ALU.mult)
        self.ps_rr = 3
        self.whold = None
        for og in range(4):
            slot = self.wget(self.pc[("aout", j, og)])
            for dd in range(4):
                d = og * 4 + dd
                ps = self.next_bank()
                for kc in range(16):
                    self.mm(ps, slot.idx(kc).cols(dd * 128, (dd + 1) * 128), O.idx(kc), kc == 0, kc == 15)
                self.tt("dve", R.idx(d), ps, R.idx(d), ALU.add)


_CACHE = {}


def get_prog(nseq, layers):
    key = (nseq, tuple(layers))
    if key not in _CACHE:
        b = Builder(nseq=nseq, layers=layers)
        _CACHE[key] = b.build()
    return _CACHE[key]


def make_inputs(inputs, nseq, ncores):
    x = np.ascontiguousarray(np.asarray(inputs["x"], np.float32))
    shared = {"vecs": build_vecs(inputs), "consts": build_consts()}
    for name in ("mlp_w1", "mlp_w2", "conv_in", "conv_out", "attn_in", "attn_out"):
        shared[name] = np.ascontiguousarray(np.asarray(inputs[name], np.float32))
    shared["w_uq"] = np.ascontiguousarray(np.asarray(inputs["w_uq"], np.float32)).reshape(2, 512, 2048)
    shared["w_qidx"] = np.ascontiguousarray(np.asarray(inputs["w_qidx"], np.float32)).reshape(2, 512, 1024)
    shared["w_uk"] = np.ascontiguousarray(np.asarray(inputs["w_uk"], np.float32))
    shared["w_uv"] = np.ascontiguousarray(np.asarray(inputs["w_uv"], np.float32))
    in_maps = []
    for c in range(ncores):
        m = dict(shared)
        m["x"] = x[c * nseq:(c + 1) * nseq].reshape(nseq * SEQ, D)
        in_maps.append(m)
    return in_maps


def run(inputs, nseq=2, layers=(0, 1, 2, 3), ncores=NCORES):
    nc = get_prog(nseq, layers)
    in_maps = make_inputs(inputs, nseq, ncores)
    if not layers:
        in_maps = [{k: v for k, v in m.items() if k in ("x", "vecs", "consts")} for m in in_maps]
    elif not any(l % 2 == 1 for l in layers):
        in_maps = [{k: v for k, v in m.items() if not (k.startswith("w_") or k.startswith("attn"))} for m in in_maps]
    res = run_bass_kernel_spmd(nc, in_maps, core_ids=list(range(ncores)))
    out = np.stack([np.asarray(r["y"]).reshape(nseq, SEQ, D) for r in res.results], 0)
    return out.reshape(ncores * nseq, SEQ, D).astype(np.float32)


def kernel(**inputs):
    return run(inputs)
```
